# Optimizing a Trainium2 kernel written in Bass

```python
import jax
import jax.numpy as jnp
from jax import lax
import numpy as np

D_MODEL = 2048
BATCH = 4
SEQ = 2048
DEPTH = 2

GRID_W = 64
CTX_LEN = 256
MLA_HEADS = 16
Q_LORA = 512
KV_LORA = 512
QK_NOPE = 128
QK_ROPE = 64
V_DIM = 128
ROPE_BASE = 10000.0
Q_BLOCK = 128
CONV_CH = 1024
CONV_WIDTH = 31
FOURIER_GROUPS = 4
FOURIER_GROUP_CH = 256
FOURIER_CH = FOURIER_GROUPS * FOURIER_GROUP_CH
N_BRANCH = 3
N_EXPERTS = 32
TOP_K = 4
D_EXPERT = 1024
SWIGLU_ALPHA = 1.702
SWIGLU_LIMIT = 7.0
EXPERT_BLOCK = 128
EPS = 1e-6
IN_OFFSETS = (Q_LORA, Q_LORA + KV_LORA, Q_LORA + KV_LORA + QK_ROPE,
              Q_LORA + KV_LORA + QK_ROPE + 2 * CONV_CH,
              Q_LORA + KV_LORA + QK_ROPE + 2 * CONV_CH + FOURIER_CH)
IN_COLS = IN_OFFSETS[-1] + N_BRANCH * D_MODEL

kernel_name = "hybrid_mla_conformer_fnet_moe_dit"


def rms_norm(x, g):
    xf = x.astype(jnp.float32)
    y = xf * lax.rsqrt(jnp.mean(xf * xf, axis=-1, keepdims=True) + EPS)
    return (y * g.astype(jnp.float32)).astype(x.dtype)


def modulate(x, g, shift, scale):
    return rms_norm(x, g) * (1 + scale) + shift


def axial_rope(rows, dtype):
    row = jnp.repeat(jnp.arange(rows, dtype=jnp.float32), GRID_W)
    col = jnp.tile(jnp.arange(GRID_W, dtype=jnp.float32), rows)
    half = QK_ROPE // 2
    inv = ROPE_BASE ** (-jnp.arange(0, half, 2, dtype=jnp.float32) / half)
    ang = jnp.concatenate([row[:, None] * inv, col[:, None] * inv], axis=-1)
    return jnp.cos(ang).astype(dtype), jnp.sin(ang).astype(dtype)


def apply_rope(t, cos, sin):
    t1, t2 = t[..., :QK_ROPE // 2], t[..., QK_ROPE // 2:]
    return jnp.concatenate([t1 * cos - t2 * sin, t2 * cos + t1 * sin], axis=-1)


def mla_q(cq, p):
    b, n, _ = cq.shape
    q = (rms_norm(cq, p['q_norm']) @ p['w_uq']).reshape(b, n, MLA_HEADS, QK_NOPE + QK_ROPE)
    return q[..., :QK_NOPE], q[..., QK_NOPE:]


def mla_kv(ckv, p):
    b, n, _ = ckv.shape
    kv = (rms_norm(ckv, p['kv_norm']) @ p['w_ukv']).reshape(b, n, MLA_HEADS, QK_NOPE + V_DIM)
    return kv[..., :QK_NOPE], kv[..., QK_NOPE:]


def mla_attend(qn, qr, kn, kr, v):
    scale = (QK_NOPE + QK_ROPE) ** -0.5
    s = jnp.einsum('bqhd,bkhd->bhqk', qn, kn) + jnp.einsum('bqhr,bkr->bhqk', qr, kr)
    prob = jax.nn.softmax(s.astype(jnp.float32) * scale, axis=-1).astype(v.dtype)
    return jnp.einsum('bhqk,bkhd->bqhd', prob, v)


def mla_latent(qn, qr, kn, kr, v):
    b, n, h, _ = qn.shape
    nb = n // Q_BLOCK

    def blocks(t):
        return jnp.moveaxis(t.reshape(b, nb, Q_BLOCK, *t.shape[2:]), 1, 0)

    out = lax.map(lambda a: mla_attend(a[0], a[1], kn, kr, v), (blocks(qn), blocks(qr)))
    return jnp.moveaxis(out, 0, 1).reshape(b, n, h * V_DIM)


def conformer_conv(u, p):
    a, g = jnp.split(u, 2, axis=-1)
    z = a * jax.nn.sigmoid(g)
    z = lax.conv_general_dilated(
        z, p['conv_dw'][:, None, :], window_strides=(1,),
        padding=[(CONV_WIDTH // 2, CONV_WIDTH // 2)],
        dimension_numbers=('NWC', 'WIO', 'NWC'), feature_group_count=CONV_CH) + p['conv_dw_b']
    zf = z.astype(jnp.float32)
    mu = jnp.mean(zf, axis=-1, keepdims=True)
    var = jnp.mean(jnp.square(zf - mu), axis=-1, keepdims=True)
    zn = (zf - mu) * lax.rsqrt(var + EPS) * p['conv_ln_g'].astype(jnp.float32) + p['conv_ln_b'].astype(jnp.float32)
    return jax.nn.silu(zn).astype(u.dtype)


def fourier_mix(f):
    b, n, _ = f.shape
    z = f.astype(jnp.float32).reshape(b, n, FOURIER_GROUPS, FOURIER_GROUP_CH)
    z = jnp.fft.fftn(z, axes=(1, 3), norm='ortho').real
    return z.reshape(b, n, FOURIER_CH).astype(f.dtype)


def merge_branches(a, u, f, gt, p):
    y_att = a @ p['w_mla_out']
    y_conv = conformer_conv(u, p) @ p['w_conv_out']
    y_four = fourier_mix(f) @ p['w_four_out']
    gs = jax.nn.sigmoid(gt).reshape(*gt.shape[:-1], N_BRANCH, D_MODEL)
    m = gs[..., 0, :] * y_att + gs[..., 1, :] * y_conv + gs[..., 2, :] * y_four
    return m @ p['w_out']


def moe_ffn(t, p):
    n_tok, d = t.shape
    logits = (t @ p['w_router']).astype(jnp.float32) + p['b_router'].astype(jnp.float32)
    top_v, top_i = lax.top_k(logits, TOP_K)
    gates = jax.nn.softmax(top_v, axis=-1).astype(t.dtype)
    n_pair = n_tok * TOP_K
    e_flat = top_i.reshape(n_pair)
    tok_flat = jnp.repeat(jnp.arange(n_tok, dtype=jnp.int32), TOP_K)
    g_flat = gates.reshape(n_pair)
    order = jnp.argsort(e_flat)
    e_sorted = e_flat[order]
    counts = jnp.bincount(e_flat, length=N_EXPERTS)
    starts = jnp.cumsum(counts) - counts
    padded = (counts + EXPERT_BLOCK - 1) // EXPERT_BLOCK * EXPERT_BLOCK
    pends = jnp.cumsum(padded)
    pstarts = pends - padded
    dest = pstarts[e_sorted] + jnp.arange(n_pair, dtype=jnp.int32) - starts[e_sorted]
    n_blocks = -(-n_pair // EXPERT_BLOCK) + N_EXPERTS
    n_rows = n_blocks * EXPERT_BLOCK
    tok_buf = jnp.zeros((n_rows,), jnp.int32).at[dest].set(tok_flat[order])
    gate_buf = jnp.zeros((n_rows,), t.dtype).at[dest].set(g_flat[order])
    block_e = jnp.minimum(
        jnp.searchsorted(pends, jnp.arange(n_blocks, dtype=jnp.int32) * EXPERT_BLOCK, side='right'),
        N_EXPERTS - 1)
    xb = t[tok_buf].reshape(n_blocks, EXPERT_BLOCK, d)

    def expert_block(args):
        xe, e = args
        hgu = xe @ p['w_gate_up'][e] + p['b_gate_up'][e]
        glu, lin = jnp.split(hgu, 2, axis=-1)
        glu = jnp.minimum(glu, SWIGLU_LIMIT)
        lin = jnp.clip(lin, -SWIGLU_LIMIT, SWIGLU_LIMIT)
        act = glu * jax.nn.sigmoid(SWIGLU_ALPHA * glu) * (lin + 1)
        return act @ p['w_down'][e] + p['b_down'][e]

    yb = lax.map(expert_block, (xb, block_e))
    y = yb.reshape(n_rows, d) * gate_buf[:, None]
    return jnp.zeros_like(t).at[tok_buf].add(y)


def trunk_layer(xl, xc, c_lat, c_ctx, p, cos, sin, last):
    b, n, d = xl.shape
    ctx_len = xc.shape[1]
    mod_l = jax.nn.silu(c_lat) @ p['w_ada'] + p['b_ada']
    mod_c = jax.nn.silu(c_ctx) @ p['w_ada'] + p['b_ada']
    sh1_l, sc1_l, g1_l, sh2_l, sc2_l, g2_l = jnp.split(mod_l[:, None, :], 6, axis=-1)
    sh1_c, sc1_c, g1_c, sh2_c, sc2_c, g2_c = jnp.split(mod_c, 6, axis=-1)

    hl = modulate(xl, p['norm1'], sh1_l, sc1_l)
    hc = modulate(xc, p['norm1'], sh1_c, sc1_c)
    cq_l, ckv_l, kr_l, u_l, f_l, gt_l = jnp.split(hl @ p['w_in'], IN_OFFSETS, axis=-1)
    if last:
        kv_cols = p['w_in'][:, Q_LORA:Q_LORA + KV_LORA + QK_ROPE]
        ckv_c, kr_c = jnp.split(hc @ kv_cols, [KV_LORA], axis=-1)
    else:
        cq_c, ckv_c, kr_c, u_c, f_c, gt_c = jnp.split(hc @ p['w_in'], IN_OFFSETS, axis=-1)
    kn_l, v_l = mla_kv(ckv_l, p)
    kr_l = apply_rope(kr_l, cos, sin)
    qn_l, qr_l = mla_q(cq_l, p)
    qr_l = apply_rope(qr_l, cos[:, None, :], sin[:, None, :])
    kn_c, v_c = mla_kv(ckv_c, p)
    kn = jnp.concatenate([kn_c, kn_l], axis=1)
    kr = jnp.concatenate([kr_c, kr_l], axis=1)
    v = jnp.concatenate([v_c, v_l], axis=1)
    a_l = mla_latent(qn_l, qr_l, kn, kr, v)
    xl = xl + g1_l * merge_branches(a_l, u_l, f_l, gt_l, p)
    if not last:
        qn_c, qr_c = mla_q(cq_c, p)
        a_c = mla_attend(qn_c, qr_c, kn_c, kr_c, v_c).reshape(b, ctx_len, MLA_HEADS * V_DIM)
        xc = xc + g1_c * merge_branches(a_c, u_c, f_c, gt_c, p)

    h2l = modulate(xl, p['norm2'], sh2_l, sc2_l).reshape(b * n, d)
    if last:
        xl = xl + g2_l * moe_ffn(h2l, p).reshape(b, n, d)
    else:
        h2c = modulate(xc, p['norm2'], sh2_c, sc2_c).reshape(b * ctx_len, d)
        y = moe_ffn(jnp.concatenate([h2l, h2c], axis=0), p)
        xl = xl + g2_l * y[:b * n].reshape(b, n, d)
        xc = xc + g2_c * y[b * n:].reshape(b, ctx_len, d)
    return xl, xc


def setup_inputs(seed: int = 0) -> dict:
    key = jax.random.key(seed)
    ks = jax.random.split(key, 32)
    f32 = jnp.float32

    def nrm(k, shape, scale):
        return scale * jax.random.normal(k, shape, f32)

    D, L = D_MODEL, DEPTH
    return {
        'x': nrm(ks[0], (BATCH, SEQ, D), 1.0),
        'c': nrm(ks[1], (BATCH, D), 1.0),
        'ctx': nrm(ks[2], (BATCH, CTX_LEN, D), 1.0),
        'c_ctx': nrm(ks[3], (D,), 1.0),
        'w_ada': nrm(ks[4], (L, D, 6 * D), 0.5 * D ** -0.5),
        'b_ada': nrm(ks[5], (L, 6 * D), 0.02),
        'norm1': 1.0 + nrm(ks[6], (L, D), 0.1),
        'w_in': nrm(ks[7], (L, D, IN_COLS), D ** -0.5),
        'q_norm': 1.0 + nrm(ks[8], (L, Q_LORA), 0.1),
        'kv_norm': 1.0 + nrm(ks[9], (L, KV_LORA), 0.1),
        'w_uq': nrm(ks[10], (L, Q_LORA, MLA_HEADS * (QK_NOPE + QK_ROPE)), Q_LORA ** -0.5),
        'w_ukv': nrm(ks[11], (L, KV_LORA, MLA_HEADS * (QK_NOPE + V_DIM)), KV_LORA ** -0.5),
        'w_mla_out': nrm(ks[12], (L, MLA_HEADS * V_DIM, D), (MLA_HEADS * V_DIM) ** -0.5),
        'conv_dw': nrm(ks[13], (L, CONV_WIDTH, CONV_CH), CONV_WIDTH ** -0.5),
        'conv_dw_b': nrm(ks[14], (L, CONV_CH), 0.02),
        'conv_ln_g': 1.0 + nrm(ks[15], (L, CONV_CH), 0.1),
        'conv_ln_b': nrm(ks[16], (L, CONV_CH), 0.02),
        'w_conv_out': nrm(ks[17], (L, CONV_CH, D), CONV_CH ** -0.5),
        'w_four_out': nrm(ks[18], (L, FOURIER_CH, D), FOURIER_CH ** -0.5),
        'w_out': nrm(ks[19], (L, D, D), D ** -0.5),
        'norm2': 1.0 + nrm(ks[20], (L, D), 0.1),
        'w_router': nrm(ks[21], (L, D, N_EXPERTS), D ** -0.5),
        'b_router': nrm(ks[22], (L, N_EXPERTS), 0.01),
        'w_gate_up': nrm(ks[23], (L, N_EXPERTS, D, 2 * D_EXPERT), D ** -0.5),
        'b_gate_up': nrm(ks[24], (L, N_EXPERTS, 2 * D_EXPERT), 0.02),
        'w_down': nrm(ks[25], (L, N_EXPERTS, D_EXPERT, D), D_EXPERT ** -0.5),
        'b_down': nrm(ks[26], (L, N_EXPERTS, D), 0.02),
        'norm_final': 1.0 + nrm(ks[27], (D,), 0.1),
    }


def reference(x, c, ctx, c_ctx, w_ada, b_ada, norm1, w_in, q_norm, kv_norm, w_uq, w_ukv,
              w_mla_out, conv_dw, conv_dw_b, conv_ln_g, conv_ln_b, w_conv_out, w_four_out,
              w_out, norm2, w_router, b_router, w_gate_up, b_gate_up, w_down, b_down,
              norm_final):
    n = x.shape[1]
    rows = n // GRID_W
    cos, sin = axial_rope(rows, x.dtype)
    xl, xc = x, ctx
    for i in range(DEPTH):
        p = {
            'w_ada': w_ada[i], 'b_ada': b_ada[i], 'norm1': norm1[i], 'w_in': w_in[i],
            'q_norm': q_norm[i], 'kv_norm': kv_norm[i], 'w_uq': w_uq[i], 'w_ukv': w_ukv[i],
            'w_mla_out': w_mla_out[i], 'conv_dw': conv_dw[i], 'conv_dw_b': conv_dw_b[i],
            'conv_ln_g': conv_ln_g[i], 'conv_ln_b': conv_ln_b[i], 'w_conv_out': w_conv_out[i],
            'w_four_out': w_four_out[i], 'w_out': w_out[i], 'norm2': norm2[i],
            'w_router': w_router[i], 'b_router': b_router[i], 'w_gate_up': w_gate_up[i],
            'b_gate_up': b_gate_up[i], 'w_down': w_down[i], 'b_down': b_down[i],
        }
        xl, xc = trunk_layer(xl, xc, c, c_ctx, p, cos, sin, i == DEPTH - 1)
    return rms_norm(xl, norm_final)
```

```python
import math
import numpy as np
import ml_dtypes
import concourse.bass as bass
import concourse.mybir as mybir
from concourse.bass_utils import run_bass_kernel_spmd

F32 = mybir.dt.float32
BF16 = mybir.dt.bfloat16
ALU = mybir.AluOpType
AF = mybir.ActivationFunctionType
AX = mybir.AxisListType

NCORES = 8
D = 2048
KC = 16
B = 4
SEQ = 2048
CTX = 256
TS = CTX + SEQ
HALF = 1024
TOWN = CTX + HALF
L = 2
NE = 32
DE = 1024
HEADS = 16
INC = 10304
O_CQ, O_CKV, O_KR, O_U, O_F, O_G = 0, 512, 1024, 1088, 3136, 4160
EPS = 1e-6
ATT_SCALE = 192 ** -0.5
ADA_SH = 12288 // 8


class Rec:
    __slots__ = ("w", "r")

    def __init__(self):
        self.w = None
        self.r = {}


class Sched:
    def __init__(self, nc):
        self.nc = nc
        self.eng = {"pe": nc.tensor, "act": nc.scalar, "dve": nc.vector, "pool": nc.gpsimd, "sp": nc.sync}
        self.sem = {k: nc.alloc_semaphore("s_" + k) for k in ["pe", "act", "dve", "pool"]}
        self.cnt = {k: 0 for k in self.sem}
        self.ND = 36
        self.dsem = [nc.alloc_semaphore(f"d{i}") for i in range(self.ND)]
        self.duse = [0] * self.ND
        self.dnext = 0
        self.csem = []
        self.waited = {k: {} for k in self.eng}
        self.state = {}
        self.nps = 0

    def _semof(self, k):
        if isinstance(k, tuple):
            return self.dsem[k[1]] if k[0] == "d" else self.csem[k[1]]
        return self.sem[k]

    def _norm(self, key):
        return key if isinstance(key, tuple) else (key, None)

    def _recs(self, key):
        name, idx = self._norm(key)
        d = self.state.setdefault(name, {})
        if idx is None:
            return list(d.values())
        out = []
        if idx in d:
            out.append(d[idx])
        if None in d:
            out.append(d[None])
        return out

    def _deps(self, reads, writes):
        toks = {}

        def add(t):
            if t is None:
                return
            k, v = t
            if toks.get(k, 0) < v:
                toks[k] = v
        for key in reads:
            for rec in self._recs(key):
                add(rec.w)
        for key in writes:
            for rec in self._recs(key):
                add(rec.w)
                for k, v in rec.r.items():
                    add((k, v))
        return toks

    def _record(self, reads, writes, tok):
        for key in reads:
            name, idx = self._norm(key)
            d = self.state.setdefault(name, {})
            rec = d.setdefault(idx, Rec())
            if rec.r.get(tok[0], 0) < tok[1]:
                rec.r[tok[0]] = tok[1]
        for key in writes:
            name, idx = self._norm(key)
            d = self.state.setdefault(name, {})
            if idx is None:
                d.clear()
            rec = Rec()
            rec.w = tok
            d[idx] = rec

    def _wait(self, E, toks):
        w = self.waited[E]
        for k, v in toks.items():
            if k == E and E == "pe":
                continue
            if w.get(k, 0) >= v:
                continue
            self.eng[E].wait_ge(self._semof(k), v)
            w[k] = v

    def op(self, E, fn, reads=(), writes=()):
        self._wait(E, self._deps(reads, writes))
        ins = fn()
        self.cnt[E] += 1
        ins.then_inc(self.sem[E], 1)
        self._record(reads, writes, (E, self.cnt[E]))
        return ins

    def mm(self, out, pairs, reads, writes, start=True, stop=True):
        self._wait("pe", self._deps(reads, writes))
        n = len(pairs)
        ins = None
        for i, (lt, rh) in enumerate(pairs):
            ins = self.nc.tensor.matmul(out, lt, rh, start=(start and i == 0), stop=(stop and i == n - 1))
        self.cnt["pe"] += 1
        ins.then_inc(self.sem["pe"], 1)
        self._record(reads, writes, ("pe", self.cnt["pe"]))

    def transpose(self, out, in_, ident, reads, writes):
        self._wait("pe", self._deps(reads, writes))
        ins = self.nc.tensor.transpose(out, in_, ident)
        self.cnt["pe"] += 1
        ins.then_inc(self.sem["pe"], 1)
        self._record(reads, writes, ("pe", self.cnt["pe"]))

    def dma(self, Q, out, in_, reads=(), writes=()):
        toks = self._deps(reads, writes)
        i = self.dnext
        self.dnext = (self.dnext + 1) % self.ND
        if self.duse[i] > 0:
            k = ("d", i)
            if toks.get(k, 0) < 16 * self.duse[i]:
                toks[k] = 16 * self.duse[i]
        self._wait(Q, toks)
        self.duse[i] += 1
        self.eng[Q].dma_start(out=out, in_=in_).then_inc(self.dsem[i], 16)
        self._record(reads, writes, (("d", i), 16 * self.duse[i]))

    def allgather(self, src, dst, reads, writes, qos=None):
        self._wait("pool", self._deps(reads, writes))
        i = len(self.csem)
        self.csem.append(self.nc.alloc_semaphore(f"c{i}"))
        self.nc.gpsimd.collective_compute(
            "AllGather", ALU.bypass, replica_groups=[list(range(NCORES))],
            ins=[src.opt()], outs=[dst.opt()], dma_qos=qos).then_inc(self.csem[i])
        self._record(reads, writes, (("c", i), 1))

    def barrier(self, engines=None):
        toks = {k: v for k, v in self.cnt.items() if v > 0}
        for i in range(self.ND):
            if self.duse[i]:
                toks[("d", i)] = 16 * self.duse[i]
        for E in (engines or self.eng):
            t = dict(toks)
            t.pop(E, None)
            if E == "pe":
                pass
            self._wait(E, t)
        if engines is None:
            keep = {}
            for name, d in self.state.items():
                for idx, rec in d.items():
                    if rec.w is not None and isinstance(rec.w[0], tuple) and rec.w[0][0] == "c":
                        r2 = Rec()
                        r2.w = rec.w
                        keep.setdefault(name, {})[idx] = r2
            self.state = keep


class Builder:
    def __init__(self, debug_stage=None, nlayers=L, with_moe=True):
        self.debug_stage = debug_stage
        self.nlayers = nlayers
        self.with_moe = with_moe
        self.nc = bass.Bass("TRN2", target_bir_lowering=False)
        self.s = Sched(self.nc)
        self.inputs = {}
        self.psn = 0

    def ext(self, name, shape, dt=F32):
        ap = self.nc.dram_tensor(name, list(shape), dt, kind="ExternalInput").ap()
        self.inputs[name] = (tuple(shape), dt)
        return ap

    def dram(self, name, shape, dt=F32):
        ap = self.nc.dram_tensor(name, list(shape), dt).ap()
        if not hasattr(self, "scratch"):
            self.scratch = {}
        self.scratch[name] = (ap, tuple(shape), dt)
        return ap

    def sb(self, stack, name, shape, dt):
        self.uid = getattr(self, "uid", 0) + 1
        return stack.enter_context(self.nc.sbuf_tensor(f"sb{self.uid}_{name}", list(shape), dt))

    def next_ps(self):
        n = getattr(self, "ps_n", 7)
        i = self.psn % n
        self.psn += 1
        return self.ps[i], ("ps", i)

    def gather_weight(self, name, shape):
        R, C = shape
        e = self.ext(name, shape)
        src = self.dram(name + "_src", shape, BF16)
        dst = self.dram(name + "_all", (NCORES * R, C), BF16)
        self.s.dma("pool", src, e, reads=[], writes=[name + "_src"])
        self.pending_ag.append((src, dst, name))
        return dst

    def flush_gathers(self, qos=None):
        for src, dst, name in self.pending_ag:
            self.s.allgather(src, dst, reads=[name + "_src"], writes=[name + "_all"], qos=qos)
        self.pending_ag = []

    def load_w(self, wt, W, c0, ncols, kc, key, wkey):
        self.wload(wt[:, 0:kc, 0:ncols], W[:, c0:c0 + ncols], key, wkey)

    def wload(self, dst, src2d, key, wkey):
        self.s.dma("sp", dst, src2d.rearrange("(kc p) n -> p kc n", p=128), reads=[wkey, key], writes=[key])

    def tok_blocks(self, t0, t1, bs=512):
        out = []
        t = t0
        while t < t1:
            n = min(bs, t1 - t)
            out.append((t, n))
            t += n
        return out


from contextlib import ExitStack


def build_program(bd, dbg=None):
    nc, s = bd.nc, bd.s
    bd.ps = [nc.alloc_psum_tensor(f"ps{i}", [128, 512], F32) for i in range(7)]
    psT = nc.alloc_psum_tensor("psT", [128, 1024], BF16)

    x_loc = bd.ext("x_loc", (TS, D))
    cT_e = bd.ext("cT", (128, KC * 5))
    onehot_e = bd.ext("onehot_b", (128, 4))
    sel8_e = bd.ext("sel8", (128, 8))
    ropeC_e = bd.ext("ropeC", (64, TS))
    ropeS_e = bd.ext("ropeS", (64, TS))
    halo_e = bd.ext("halo_mask", (128, 2))
    posC_e = bd.ext("posC", (SEQ, HALF), BF16)
    posS_e = bd.ext("posS", (SEQ, HALF), BF16)
    cposC_e = bd.ext("cposC", (CTX, CTX), BF16)
    cposS_e = bd.ext("cposS", (CTX, CTX), BF16)
    chC_e = bd.ext("chC", (256, 256), BF16)
    chSn_e = bd.ext("chSn", (256, 256), BF16)
    identb_e = bd.ext("ident_bf", (128, 128), BF16)
    identf_e = bd.ext("ident_f32", (128, 128))
    norm1_e = bd.ext("norm1_pp", (L, 128, KC))
    norm2_e = bd.ext("norm2_pp", (L, 128, KC))
    qn_e = bd.ext("qnorm_pp", (L, 128, 4))
    kvn_e = bd.ext("kvnorm_pp", (L, 128, 4))
    cdw_e = bd.ext("conv_dw_pp", (L, 128, 8 * 31))
    cdb_e = bd.ext("conv_b_pp", (L, 128, 8))
    clg_e = bd.ext("conv_lng_pp", (L, 128, 8))
    clb_e = bd.ext("conv_lnb_pp", (L, 128, 8))
    bgu_e = bd.ext("b_gu_pp", (L, 128, NE * 16))
    brt_e = bd.ext("b_router_bc", (L, 128, NE))
    bdn_e = bd.ext("b_down", (L, NE, D))
    wrt_e = bd.ext("w_router_pp", (L, 128, KC * NE))
    nfin_e = bd.ext("norm_final_bc", (128, D))
    bada_e = bd.ext("b_ada_sh", (L, 1, ADA_SH))
    wada_e = bd.ext("w_ada_sh", (L, D, ADA_SH))
    out_e = nc.dram_tensor("out", [HALF, D], F32, kind="ExternalOutput").ap()

    xcur = bd.dram("xcur", (TS, D))
    xmid = bd.dram("xmid", (TOWN, D))
    hfm = bd.dram("hfm", (KC, 128, TOWN), BF16)
    mfm = bd.dram("mfm", (KC, 128, TOWN), BF16)
    h2fm = bd.dram("h2fm", (KC, 128, TOWN), BF16)
    modpart = bd.dram("modpart", (12, 10 * 128))
    modall = bd.dram("modall", (96, 10 * 128))
    xown = bd.dram("xown", (HALF, D))
    xgath = bd.dram("xgath", (NCORES * HALF, D))

    with ExitStack() as gs:
        ident_b = bd.sb(gs, "ident_b", (128, 128), BF16)
        ident_f = bd.sb(gs, "ident_f", (128, 128), F32)
        ones_b = bd.sb(gs, "ones_b", (128, 128), BF16)
        ones_f = bd.sb(gs, "ones_f", (128, 128), F32)
        onehot = bd.sb(gs, "onehot", (128, 4), F32)
        halo_m = bd.sb(gs, "halo_m", (128, 2), F32)
        modpp = bd.sb(gs, "modpp", (128, 2, 6 * KC), F32)
        s.dma("sp", ident_b[:], identb_e, writes=["ident_b"])
        s.dma("sp", ident_f[:], identf_e, writes=["ident_f"])
        s.dma("sp", onehot[:], onehot_e, writes=["onehot"])
        s.dma("sp", halo_m[:], halo_e, writes=["halo_m"])
        s.op("dve", lambda: nc.vector.memset(ones_b[:], 1.0), writes=["ones_b"])
        s.op("dve", lambda: nc.vector.memset(ones_f[:], 1.0), writes=["ones_f"])
        s.dma("sp", xcur, x_loc, writes=["xcur"])

        with ExitStack() as st:
            cT = bd.sb(st, "cT", (128, KC, 5), F32)
            scT = bd.sb(st, "scT", (128, KC, 5), BF16)
            wad = [bd.sb(st, f"wad{i}", (128, KC, 512), BF16) for i in range(2)]
            bad = bd.sb(st, "bad", (1, ADA_SH), F32)
            msb = bd.sb(st, "msb", (5, ADA_SH), F32)
            s.dma("sp", cT[:].rearrange("p k f -> p (k f)"), cT_e, writes=["cT"])
            s.op("act", lambda: nc.scalar.activation(scT[:].rearrange("p k f -> p (k f)"),
                                                     cT[:].rearrange("p k f -> p (k f)"), AF.Silu),
                 reads=["cT"], writes=["scT"])
            n = 0
            for l in range(L):
                s.dma("sp", bad[:], bada_e[l], reads=["bad"], writes=["bad"])
                for blk in range(3):
                    wt = wad[n % 2]
                    wk = ("wad", n % 2)
                    n += 1
                    s.dma("pool", wt[:, 0:KC, 0:512], wada_e[l][:, blk * 512:(blk + 1) * 512].rearrange("(kc p) n -> p kc n", p=128),
                          reads=[wk], writes=[wk])
                    ps, pk = bd.next_ps()
                    pairs = [(scT[:, kc, :], wt[:, kc, :]) for kc in range(KC)]
                    pairs.append((ones_f[0:1, 0:5], bad[0:1, blk * 512:(blk + 1) * 512]))
                    s.mm(ps[0:5, :], pairs, reads=["scT", wk, "bad", "ones_f"], writes=[pk])
                    s.op("dve", lambda ps=ps, blk=blk: nc.vector.tensor_copy(msb[:, blk * 512:(blk + 1) * 512], ps[0:5, :]),
                         reads=[pk], writes=["msb"])
                dst = modpart.rearrange("cc (q p) -> q cc p", p=128)[l * 5:(l + 1) * 5]
                s.dma("sp", dst, msb[:].rearrange("r (cc p) -> r cc p", p=128), reads=["msb"], writes=["modpart"])
            s.allgather(modpart, modall, reads=["modpart"], writes=["modall"])
            s.barrier()

        Wg = {}
        bd.pending_ag = []
        wdefs = [("w_in", D // 8, INC), ("w_uq", 64, 3072), ("w_ukv", 64, 4096), ("w_mla_out", D // 8, D),
                 ("w_conv_out", 128, D), ("w_four_out", 128, D), ("w_out", D // 8, D)]
        for l in range(bd.nlayers):
            for nm, r, c in wdefs:
                Wg[(nm, l)] = bd.gather_weight(f"{nm}_{l}", (r, c))
            if bd.with_moe:
                for j in range(4):
                    Wg[("gu", l, j)] = bd.gather_weight(f"w_gu_{l}_{j}", (D, 2 * DE))
                    Wg[("dn", l, j)] = bd.gather_weight(f"w_dn_{l}_{j}", (DE, D))
        bd.flush_gathers(qos="P3")


        for l in range(bd.nlayers):
            last = (l == L - 1)
            layer(bd, l, last, locals())
            if dbg is not None and dbg[0] == l:
                break
        if dbg is not None:
            for nm in dbg[1]:
                ap, shape, dt = bd.scratch[nm]
                o = nc.dram_tensor("dbg_" + nm, list(shape), dt, kind="ExternalOutput").ap()
                s.dma("sp", o, ap, reads=[nm], writes=["dbg_" + nm])
            s.barrier()
    return nc


def fm_view(ap3):
    return ap3.rearrange("c p t -> p c t")


def bcast_rows(ap2, nparts=128):
    n = ap2.shape[-1]
    return bass.AP(ap2.tensor, ap2.offset, [[0, nparts], [1, n]])


def layer(bd, l, last, env):
    nc, s = bd.nc, bd.s
    g = env
    Wg = g["Wg"]
    xcur, xmid, hfm, mfm, h2fm, modall = g["xcur"], g["xmid"], g["hfm"], g["mfm"], g["h2fm"], g["modall"]
    ident_b, ident_f, ones_b, ones_f = g["ident_b"], g["ident_f"], g["ones_b"], g["ones_f"]
    onehot, halo_m, modpp, psT = g["onehot"], g["halo_m"], g["modpp"], g["psT"]
    NT_OWN = TOWN // 128
    NT_ALL = TS // 128
    own_blocks = bd.tok_blocks(0, TOWN)

    cqn_d = bd.dram(f"cqn_d{l}", (4, 128, TOWN), BF16)
    ckvn_d = bd.dram(f"ckvn_d{l}", (4, 128, TS), BF16)
    kr_d = bd.dram(f"kr_d{l}", (64, TS), BF16)
    fz_d = bd.dram(f"fz_d{l}", (TS, 1024), BF16)
    convfm = bd.dram(f"convfm{l}", (8, 128, TOWN), BF16)
    fourfm = bd.dram(f"fourfm{l}", (8, 128, TOWN), BF16)
    afm = bd.dram(f"afm{l}", (16, 128, TOWN), BF16)
    modsel = bd.dram(f"modsel{l}", (2, 96 * 128))

    with ExitStack() as st:
        mrows = bd.sb(st, "mrows", (96, 5, 128), F32)
        msel = bd.sb(st, "msel", (96, 2, 128), F32)
        src = modall.rearrange("gc (q p) -> gc q p", p=128)[:, l * 5:(l + 1) * 5, :]
        s.dma("sp", mrows[:], src, reads=["modall"], writes=["mrows"])
        s.op("dve", lambda: nc.vector.tensor_scalar(msel[:, 0, :], mrows[:, 0, :], onehot[0:96, 0:1], None, ALU.mult),
             reads=["mrows", "onehot"], writes=["msel"])
        for q in range(1, 4):
            s.op("dve", lambda q=q: nc.vector.scalar_tensor_tensor(msel[:, 0, :], mrows[:, q, :], onehot[0:96, q:q + 1],
                                                                   msel[:, 0, :], ALU.mult, ALU.add),
                 reads=["mrows", "onehot", "msel"], writes=["msel"])
        s.op("dve", lambda: nc.vector.tensor_copy(msel[:, 1, :], mrows[:, 4, :]), reads=["mrows", "msel"], writes=["msel"])
        for v in range(2):
            ps, pk = bd.next_ps()
            s.transpose(ps[:, 0:96], msel[:, v, :], ident_f[0:96, 0:96], reads=["msel", "ident_f"], writes=[pk])
            s.op("dve", lambda ps=ps, v=v: nc.vector.tensor_copy(modpp[:, v, :], ps[:, 0:96]), reads=[pk], writes=["modpp"])
            s.dma("sp", modsel[v].rearrange("(r p) -> r p", p=128), msel[:, v, :], reads=["msel"], writes=["modsel"])
        s.barrier()

    def norm_T(st_key, src_ap, src_key, Dn, out_sb, out_key, tcol, A_ap, B_ap, tmp, fp32_out=None):
        sq, ss, xh = tmp
        nch = Dn // 128
        s.op("act", lambda: nc.scalar.activation(sq[:, 0:Dn], src_ap, AF.Square), reads=[src_key], writes=["n_sq"])
        s.op("dve", lambda: nc.vector.reduce_sum(ss[:, 0:1], sq[:, 0:Dn], axis=AX.X), reads=["n_sq"], writes=["n_ss"])
        s.op("dve", lambda: nc.vector.tensor_scalar(ss[:, 1:2], ss[:, 0:1], 1.0 / Dn, EPS, ALU.mult, ALU.add),
             reads=["n_ss"], writes=["n_ss"])
        s.op("act", lambda: nc.scalar.activation(ss[:, 2:3], ss[:, 1:2], AF.Sqrt), reads=["n_ss"], writes=["n_ss"])
        s.op("dve", lambda: nc.vector.reciprocal(ss[:, 3:4], ss[:, 2:3]), reads=["n_ss"], writes=["n_ss"])
        if fp32_out is None:
            s.op("dve", lambda: nc.vector.tensor_scalar(xh[:, 0:Dn], src_ap, ss[:, 3:4], None, ALU.mult),
                 reads=[src_key, "n_ss"], writes=["n_xh"])
            for c0 in range(0, nch, 8):
                cn = min(8, nch - c0)
                for c in range(c0, c0 + cn):
                    s.transpose(psT[:, (c - c0) * 128:(c - c0 + 1) * 128], xh[:, c * 128:(c + 1) * 128], ident_b[:],
                                reads=["n_xh", "ident_b"], writes=["psT"])
                for c in range(c0, c0 + cn):
                    pin = psT[:, (c - c0) * 128:(c - c0 + 1) * 128]
                    dst = out_sb[:, c, tcol:tcol + 128]
                    if B_ap is not None:
                        s.op("dve", lambda pin=pin, dst=dst, c=c: nc.vector.tensor_scalar(
                            dst, pin, A_ap[:, c:c + 1], B_ap[:, c:c + 1], ALU.mult, ALU.add),
                            reads=["psT", "ABpp"], writes=[out_key])
                    else:
                        s.op("dve", lambda pin=pin, dst=dst, c=c: nc.vector.tensor_scalar(
                            dst, pin, A_ap[:, c:c + 1], None, ALU.mult), reads=["psT", "ABpp"], writes=[out_key])
        else:
            xf, of32 = fp32_out
            s.op("dve", lambda: nc.vector.tensor_scalar(xf[:, 0:Dn], src_ap, ss[:, 3:4], None, ALU.mult),
                 reads=[src_key, "n_ss"], writes=["n_xf"])
            for c0 in range(0, nch, 4):
                ps, pk = bd.next_ps()
                for c in range(c0, c0 + 4):
                    s.transpose(ps[:, (c - c0) * 128:(c - c0 + 1) * 128], xf[:, c * 128:(c + 1) * 128], ident_f[:],
                                reads=["n_xf", "ident_f"], writes=[pk])
                for c in range(c0, c0 + 4):
                    pin = ps[:, (c - c0) * 128:(c - c0 + 1) * 128]
                    s.op("dve", lambda pin=pin, c=c: nc.vector.tensor_scalar(
                        of32[:, c, :], pin, A_ap[:, c:c + 1], B_ap[:, c:c + 1], ALU.mult, ALU.add),
                        reads=[pk, "ABpp"], writes=["of32"])
                    s.op("act", lambda c=c: nc.scalar.copy(out_sb[:, c, tcol:tcol + 128], of32[:, c, :]),
                         reads=["of32"], writes=[out_key])

    def make_AB(st, gain_e, m_sh, m_sc):
        gpp = bd.sb(st, "gpp", (128, KC), F32)
        AB = bd.sb(st, "AB", (128, 2, 2, KC), F32)
        s.dma("sp", gpp[:], gain_e[l], reads=[], writes=["gpp"])
        for v in range(2):
            s.op("dve", lambda v=v: nc.vector.scalar_tensor_tensor(
                AB[:, v, 0, :], modpp[:, v, m_sc * KC:(m_sc + 1) * KC], 1.0, gpp[:], ALU.add, ALU.mult),
                reads=["modpp", "gpp"], writes=["ABpp"])
            s.op("dve", lambda v=v: nc.vector.tensor_copy(AB[:, v, 1, :], modpp[:, v, m_sh * KC:(m_sh + 1) * KC]),
                 reads=["modpp", "ABpp"], writes=["ABpp"])
        return AB

    def gemm_fm(W, cols, kcn, X, xkey, blocks, wring, wtag, wkey, epi):
        groups = []
        for ci, (c0, n) in enumerate(cols):
            if groups and groups[-1][0] + groups[-1][1] == c0 and groups[-1][1] + n <= 512:
                groups[-1][1] += n
                groups[-1][2].append((ci, c0, n))
            else:
                groups.append([c0, n, [(ci, c0, n)]])

        def load(gi):
            c0, n, _ = groups[gi]
            slot = bd.wslot[wtag] % len(wring)
            bd.wslot[wtag] += 1
            bd.load_w(wring[slot], W, c0, n, kcn, (wtag, slot), wkey)
            return slot
        pre = len(wring) > 1
        slots = {0: load(0)}
        for gi in range(len(groups)):
            if pre and gi + 1 < len(groups):
                slots[gi + 1] = load(gi + 1)
            if not pre and gi > 0:
                slots[gi] = load(gi)
            c0g, ng, members = groups[gi]
            wt = wring[slots[gi]]
            for (ci, c0, n) in members:
                off = c0 - c0g
                for (t0, tn) in blocks:
                    ps, pk = bd.next_ps()
                    pairs = [(wt[:, kc, off:off + n], X[:, kc, t0:t0 + tn]) for kc in range(kcn)]
                    s.mm(ps[0:n, 0:tn], pairs, reads=[(wtag, slots[gi]), xkey], writes=[pk])
                    epi(ci, (t0, tn), ps, pk)

    def gemm_tm(W, c0, ncols, kcn, X, xkey, tiles, wring, wtag, wkey, epi):
        groups = [(c, min(512, c0 + ncols - c)) for c in range(c0, c0 + ncols, 512)]

        def load(gi):
            cg0, n = groups[gi]
            slot = bd.wslot[wtag] % len(wring)
            bd.wslot[wtag] += 1
            bd.load_w(wring[slot], W, cg0, n, kcn, (wtag, slot), wkey)
            return slot
        pre = len(wring) > 1
        slots = {0: load(0)}
        for gi in range(len(groups)):
            if pre and gi + 1 < len(groups):
                slots[gi + 1] = load(gi + 1)
            if not pre and gi > 0:
                slots[gi] = load(gi)
            cg0, n = groups[gi]
            wt = wring[slots[gi]]
            for t in tiles:
                ps, pk = bd.next_ps()
                pairs = [(X[:, kc, t * 128:(t + 1) * 128], wt[:, kc, 0:n]) for kc in range(kcn)]
                s.mm(ps[:, 0:n], pairs, reads=[(wtag, slots[gi]), xkey], writes=[pk])
                epi(gi, t, ps, pk, cg0, n)

    bd.wslot = {"w16": 0, "w4": 0, "wm": 0, "wgu": 0, "wdn": 0, "wf": 0}
    W_in = Wg[("w_in", l)]

    with ExitStack() as st:
        hT = bd.sb(st, "hT", (128, KC, TS), BF16)
        w16 = [bd.sb(st, f"w16_{i}", (128, KC, 512), BF16) for i in range(2)]
        AB = make_AB(st, g["norm1_e"], 0, 1)
        with ExitStack() as st2:
            xt = [bd.sb(st2, f"xt{i}", (128, D), F32) for i in range(2)]
            sq = bd.sb(st2, "sq", (128, D), F32)
            ss = bd.sb(st2, "ss", (128, 4), F32)
            xh = bd.sb(st2, "xh", (128, D), BF16)
            for t in range(NT_ALL):
                xb = xt[t % 2]
                s.dma("sp", xb[:], xcur[t * 128:(t + 1) * 128, :], reads=["xcur"], writes=[("xt", t % 2)])
                v = 1 if t < 2 else 0
                norm_T(st2, xb[:], ("xt", t % 2), D, hT, "hT", t * 128, AB[:, v, 0, :], AB[:, v, 1, :], (sq, ss, xh))
            s.barrier()
        s.dma("sp", fm_view(hfm), hT[:, :, 0:TOWN], reads=["hT"], writes=["hfm"])

        with ExitStack() as st2:
            sq = bd.sb(st2, "sq", (128, 512), F32)
            ss = bd.sb(st2, "ss", (128, 4), F32)
            xh = bd.sb(st2, "xh", (128, 512), BF16)
            qg = bd.sb(st2, "qg", (128, 2, 4), F32)
            s.dma("sp", qg[:, 0, :], g["qn_e"][l], writes=["ABpp"])
            s.dma("sp", qg[:, 1, :], g["kvn_e"][l], reads=["ABpp"], writes=["ABpp"])
            stg = bd.sb(st2, "stg", (128, 4, TS), BF16)
            gemm_tm(W_in, O_CQ, 512, KC, hT, "hT", range(NT_OWN), w16, "w16", "w_in_%d_all" % l,
                    lambda gi, t, ps, pk, c0, n: norm_T(st2, ps[:, 0:512], pk, 512, stg, "stg", t * 128, qg[:, 0, :], None, (sq, ss, xh)))
            s.dma("sp", fm_view(cqn_d), stg[:, :, 0:TOWN], reads=["stg"], writes=["cqn_d"])
            gemm_tm(W_in, O_CKV, 512, KC, hT, "hT", range(NT_ALL), w16, "w16", "w_in_%d_all" % l,
                    lambda gi, t, ps, pk, c0, n: norm_T(st2, ps[:, 0:512], pk, 512, stg, "stg", t * 128, qg[:, 1, :], None, (sq, ss, xh)))
            s.dma("sp", fm_view(ckvn_d), stg[:, :, :], reads=["stg"], writes=["ckvn_d"])
            s.barrier()

        with ExitStack() as st2:
            wkr = bd.sb(st2, "wkr", (128, KC, 128), BF16)
            rC = bd.sb(st2, "rC", (64, TS), F32)
            rS = bd.sb(st2, "rS", (64, TS), F32)
            krs = bd.sb(st2, "krs", (64, TS), BF16)
            tmp = [bd.sb(st2, f"krt{i}", (64, 512), F32) for i in range(2)]
            s.dma("sp", rC[:], g["ropeC_e"], writes=["rC"])
            s.dma("sp", rS[:], g["ropeS_e"], writes=["rS"])
            wk = "w_in_%d_all" % l
            for (dst0, src0, n) in ((0, O_KR, 64), (64, O_KR + 32, 32), (96, O_KR, 32)):
                bd.wload(wkr[:, :, dst0:dst0 + n], W_in[:, src0:src0 + n], "wkr", wk)
            for (t0, tn) in bd.tok_blocks(0, TS):
                p1, k1 = bd.next_ps()
                p2, k2 = bd.next_ps()
                s.mm(p1[0:64, 0:tn], [(wkr[:, kc, 0:64], hT[:, kc, t0:t0 + tn]) for kc in range(KC)], reads=["wkr", "hT"], writes=[k1])
                s.mm(p2[0:64, 0:tn], [(wkr[:, kc, 64:128], hT[:, kc, t0:t0 + tn]) for kc in range(KC)], reads=["wkr", "hT"], writes=[k2])
                s.op("dve", lambda p1=p1, t0=t0, tn=tn: nc.vector.tensor_tensor(tmp[0][:, 0:tn], p1[0:64, 0:tn], rC[:, t0:t0 + tn], ALU.mult),
                     reads=[k1, "rC"], writes=["krt0"])
                s.op("dve", lambda p2=p2, t0=t0, tn=tn: nc.vector.tensor_tensor(tmp[1][:, 0:tn], p2[0:64, 0:tn], rS[:, t0:t0 + tn], ALU.mult),
                     reads=[k2, "rS"], writes=["krt1"])
                s.op("dve", lambda t0=t0, tn=tn: nc.vector.tensor_tensor(krs[:, t0:t0 + tn], tmp[0][:, 0:tn], tmp[1][:, 0:tn], ALU.add),
                     reads=["krt0", "krt1"], writes=["krs"])
            s.dma("sp", kr_d, krs[:], reads=["krs"], writes=["kr_d"])
            s.barrier()

        with ExitStack() as st2:
            fst = [bd.sb(st2, f"fst{i}", (128, 512), BF16) for i in range(2)]
            cnt = [0]

            def epi_f(gi, t, ps, pk, c0, n):
                b_ = fst[cnt[0] % 2]
                k_ = ("fst", cnt[0] % 2)
                cnt[0] += 1
                s.op("act", lambda: nc.scalar.copy(b_[:, 0:n], ps[:, 0:n]), reads=[pk], writes=[k_])
                s.dma("sp", fz_d[t * 128:(t + 1) * 128, c0 - O_F:c0 - O_F + n], b_[:, 0:n], reads=[k_], writes=["fz_d"])
            gemm_tm(W_in, O_F, 1024, KC, hT, "hT", range(NT_ALL), w16, "w16", "w_in_%d_all" % l, epi_f)
            s.barrier()

        with ExitStack() as st2:
            acc = bd.sb(st2, "acc", (128, 8, TOWN), F32)
            zb = [bd.sb(st2, "zb0", (128, TOWN + 60), F32)] * 2
            sg = [bd.sb(st2, f"sg{i}", (128, 512), F32) for i in range(2)]
            cw = bd.sb(st2, "cw", (128, 8, 31), F32)
            cb = bd.sb(st2, "cb", (128, 3, 8), F32)
            s.dma("sp", cw[:].rearrange("p c k -> p (c k)"), g["cdw_e"][l], writes=["cw"])
            s.dma("sp", cb[:, 0, :], g["cdb_e"][l], writes=["cb"])
            s.dma("sp", cb[:, 1, :], g["clg_e"][l], reads=["cb"], writes=["cb"])
            s.dma("sp", cb[:, 2, :], g["clb_e"][l], reads=["cb"], writes=["cb"])
            ZC0, ZL0 = 15, 15 + 256 + 15 + 15
            ublocks = [(0, 256, ZC0, None), (256, 512, ZL0, None), (768, 512, ZL0 + 512, None),
                       (TS - 15, 15, ZL0 - 15, 0), (TOWN, 15, ZL0 + 1024, 1)]
            sgc = [0]
            for c in range(8):
                z = zb[c % 2]
                zk = ("zb", 0)
                s.op("dve", lambda z=z: nc.vector.memset(z[:], 0.0), reads=[], writes=[zk])
                slot = bd.wslot["w16"] % 2
                bd.wslot["w16"] += 1
                wt = w16[slot]
                bd.load_w(wt[:, :, 0:128], W_in, O_U + c * 128, 128, KC, ("w16", slot), wk)
                bd.wload(wt[:, :, 128:256], W_in[:, O_U + 1024 + c * 128:O_U + 1024 + (c + 1) * 128], ("w16", slot), wk)
                for (t0, tn, z0, hm) in ublocks:
                    pa, ka = bd.next_ps()
                    pg, kg = bd.next_ps()
                    s.mm(pa[:, 0:tn], [(wt[:, kc, 0:128], hT[:, kc, t0:t0 + tn]) for kc in range(KC)], reads=[("w16", slot), "hT"], writes=[ka])
                    s.mm(pg[:, 0:tn], [(wt[:, kc, 128:256], hT[:, kc, t0:t0 + tn]) for kc in range(KC)], reads=[("w16", slot), "hT"], writes=[kg])
                    sb_ = sg[sgc[0] % 2]
                    sk = ("sg", sgc[0] % 2)
                    sgc[0] += 1
                    s.op("act", lambda pg=pg, sb_=sb_, tn=tn: nc.scalar.activation(sb_[:, 0:tn], pg[:, 0:tn], AF.Sigmoid), reads=[kg], writes=[sk])
                    s.op("dve", lambda pa=pa, sb_=sb_, z=z, z0=z0, tn=tn: nc.vector.tensor_tensor(z[:, z0:z0 + tn], pa[:, 0:tn], sb_[:, 0:tn], ALU.mult),
                         reads=[ka, sk], writes=[zk])
                    if hm is not None:
                        s.op("dve", lambda z=z, z0=z0, tn=tn, hm=hm: nc.vector.tensor_scalar(z[:, z0:z0 + tn], z[:, z0:z0 + tn], halo_m[:, hm:hm + 1], None, ALU.mult),
                             reads=[zk, "halo_m"], writes=[zk])
                for (a0, an, zs) in ((0, 256, ZC0 - 15), (256, 1024, ZL0 - 15)):
                    s.op("dve", lambda z=z, c=c, a0=a0, an=an, zs=zs: nc.vector.tensor_scalar(
                        acc[:, c, a0:a0 + an], z[:, zs:zs + an], cw[:, c, 0:1], cb[:, 0, c:c + 1], ALU.mult, ALU.add),
                        reads=[zk, "cw", "cb"], writes=[("acc", c)])
                    for k in range(1, 31):
                        s.op("dve", lambda z=z, c=c, a0=a0, an=an, zs=zs, k=k: nc.vector.scalar_tensor_tensor(
                            acc[:, c, a0:a0 + an], z[:, zs + k:zs + k + an], cw[:, c, k:k + 1], acc[:, c, a0:a0 + an], ALU.mult, ALU.add),
                            reads=[zk, "cw", ("acc", c)], writes=[("acc", c)])
            mean = bd.sb(st2, "mean", (128, 512), F32)
            rstd = bd.sb(st2, "rstd", (128, 512), F32)
            sqc = [bd.sb(st2, f"sqc{i}", (128, 512), F32) for i in range(2)]
            cst = [bd.sb(st2, f"cst{i}", (128, 512), BF16) for i in range(2)]
            cstn = [0]
            for (t0, tn) in own_blocks:
                pm, km = bd.next_ps()
                pq, kq = bd.next_ps()
                s.mm(pm[:, 0:tn], [(ones_f[:], acc[:, c, t0:t0 + tn]) for c in range(8)], reads=["ones_f", "acc"], writes=[km])
                for c in range(8):
                    q_ = sqc[c % 2]
                    s.op("act", lambda q_=q_, c=c, t0=t0, tn=tn: nc.scalar.activation(q_[:, 0:tn], acc[:, c, t0:t0 + tn], AF.Square),
                         reads=["acc"], writes=[("sqc", c % 2)])
                    s.mm(pq[:, 0:tn], [(ones_f[:], q_[:, 0:tn])], reads=["ones_f", ("sqc", c % 2)], writes=[kq], start=(c == 0), stop=(c == 7))
                s.op("dve", lambda pm=pm, tn=tn: nc.vector.tensor_scalar(mean[:, 0:tn], pm[:, 0:tn], 1.0 / 1024, None, ALU.mult), reads=[km], writes=["mean"])
                s.op("dve", lambda tn=tn: nc.vector.tensor_tensor(rstd[:, 0:tn], mean[:, 0:tn], mean[:, 0:tn], ALU.mult), reads=["mean"], writes=["rstd"])
                s.op("dve", lambda pq=pq, tn=tn: nc.vector.scalar_tensor_tensor(rstd[:, 0:tn], pq[:, 0:tn], 1.0 / 1024, rstd[:, 0:tn], ALU.mult, ALU.subtract),
                     reads=[kq, "rstd"], writes=["rstd"])
                s.op("dve", lambda tn=tn: nc.vector.tensor_scalar(rstd[:, 0:tn], rstd[:, 0:tn], EPS, None, ALU.add), reads=["rstd"], writes=["rstd"])
                s.op("act", lambda tn=tn: nc.scalar.activation(rstd[:, 0:tn], rstd[:, 0:tn], AF.Sqrt), reads=["rstd"], writes=["rstd"])
                s.op("dve", lambda tn=tn: nc.vector.reciprocal(rstd[:, 0:tn], rstd[:, 0:tn]), reads=["rstd"], writes=["rstd"])
                for c in range(8):
                    s.op("dve", lambda c=c, t0=t0, tn=tn: nc.vector.tensor_tensor(acc[:, c, t0:t0 + tn], acc[:, c, t0:t0 + tn], mean[:, 0:tn], ALU.subtract),
                         reads=["acc", "mean"], writes=["acc"])
                    s.op("dve", lambda c=c, t0=t0, tn=tn: nc.vector.tensor_tensor(acc[:, c, t0:t0 + tn], acc[:, c, t0:t0 + tn], rstd[:, 0:tn], ALU.mult),
                         reads=["acc", "rstd"], writes=["acc"])
                    cb_ = cst[cstn[0] % 2]
                    ck_ = ("cst", cstn[0] % 2)
                    cstn[0] += 1
                    s.op("act", lambda c=c, t0=t0, tn=tn, cb_=cb_: nc.scalar.activation(cb_[:, 0:tn], acc[:, c, t0:t0 + tn], AF.Silu,
                                                                                     bias=cb[:, 2, c:c + 1], scale=cb[:, 1, c:c + 1]),
                         reads=["acc", "cb"], writes=[ck_])
                    s.dma("sp", convfm[c][:, t0:t0 + tn], cb_[:, 0:tn], reads=[ck_], writes=["convfm"])
            s.barrier()
    s.barrier()
    if bd.stop_after == ("mix1", l):
        return
    layer_part2(bd, l, last, env, locals())


def layer_part2(bd, l, last, env, loc):
    nc, s = bd.nc, bd.s
    g = env
    Wg = g["Wg"]
    xcur, xmid, hfm, mfm, h2fm = g["xcur"], g["xmid"], g["hfm"], g["mfm"], g["h2fm"]
    ident_b, ident_f, ones_b, ones_f = g["ident_b"], g["ident_f"], g["ones_b"], g["ones_f"]
    modpp, psT = g["modpp"], g["psT"]
    cqn_d, ckvn_d, kr_d, fz_d, convfm, fourfm, afm = (loc[k] for k in ("cqn_d", "ckvn_d", "kr_d", "fz_d", "convfm", "fourfm", "afm"))
    gemm_fm, gemm_tm, norm_T, make_AB, own_blocks = loc["gemm_fm"], loc["gemm_tm"], loc["norm_T"], loc["make_AB"], loc["own_blocks"]
    NT_OWN, NT_ALL = TOWN // 128, TS // 128

    with ExitStack() as st:
        fz = bd.sb(st, "fz", (128, NT_ALL, 1024), BF16)
        s.dma("sp", fz[:], fz_d.rearrange("(t p) c -> p t c", p=128), reads=["fz_d"], writes=["fz"])
        pcs = bd.sb(st, "pcs", (128, 2, 8, TOWN), BF16)
        cring = [bd.sb(st, f"cpos{i}", (128, 16, 512), BF16) for i in range(2)]
        chm = bd.sb(st, "chm", (128, 2, 2, 256), BF16)
        s.dma("sp", chm[:, 0, :, :], g["chC_e"].rearrange("(k p) m -> p k m", p=128), writes=["chm"])
        s.dma("sp", chm[:, 1, :, :], g["chSn_e"].rearrange("(k p) m -> p k m", p=128), reads=["chm"], writes=["chm"])
        n = 0
        for (cs, (Ce, Se), ntile0, nk, kblocks, kout0) in (
                ("ctx", (g["cposC_e"], g["cposS_e"]), 0, 2, [(0, 256)], 0),
                ("lat", (g["posC_e"], g["posS_e"]), 2, 16, [(0, 512), (512, 512)], 256)):
            for ti, tab in enumerate((Ce, Se)):
                for (k0, kn) in kblocks:
                    ct = cring[n % 2]
                    ck = ("cpos", n % 2)
                    n += 1
                    s.dma("sp", ct[:, 0:nk, 0:kn], tab[:, k0:k0 + kn].rearrange("(k p) m -> p k m", p=128), reads=[ck], writes=[ck])
                    for c in range(8):
                        ps, pk = bd.next_ps()
                        s.mm(ps[:, 0:kn], [(fz[:, ntile0 + j, c * 128:(c + 1) * 128], ct[:, j, 0:kn]) for j in range(nk)],
                             reads=["fz", ck], writes=[pk])
                        s.op("act", lambda ps=ps, ti=ti, c=c, k0=k0, kn=kn, kout0=kout0: nc.scalar.copy(
                            pcs[:, ti, c, kout0 + k0:kout0 + k0 + kn], ps[:, 0:kn]), reads=[pk], writes=["pcs"])
        fst = bd.sb(st, "fst", (128, 8, TOWN), BF16)
        for grp in range(4):
            for mh in range(2):
                for (t0, tn) in own_blocks:
                    ps, pk = bd.next_ps()
                    pairs = []
                    for ti in range(2):
                        for ch in range(2):
                            pairs.append((chm[:, ti, ch, mh * 128:(mh + 1) * 128], pcs[:, ti, grp * 2 + ch, t0:t0 + tn]))
                    s.mm(ps[:, 0:tn], pairs, reads=["chm", "pcs"], writes=[pk])
                    s.op("act", lambda ps=ps, grp=grp, mh=mh, t0=t0, tn=tn: nc.scalar.copy(fst[:, grp * 2 + mh, t0:t0 + tn], ps[:, 0:tn]),
                         reads=[pk], writes=["fst"])
        s.dma("sp", fm_view(fourfm), fst[:], reads=["fst"], writes=["fourfm"])
        s.barrier()

    W_uq, W_ukv = Wg[("w_uq", l)], Wg[("w_ukv", l)]
    with ExitStack() as st:
        cqn = bd.sb(st, "cqn", (128, 4, TOWN), BF16)
        ckvn = bd.sb(st, "ckvn", (128, 4, TS), BF16)
        krT = bd.sb(st, "krT", (64, TS), BF16)
        rC = bd.sb(st, "rCq", (64, TOWN), F32)
        rS = bd.sb(st, "rSq", (64, TOWN), F32)
        s.dma("sp", cqn[:], fm_view(cqn_d), reads=["cqn_d"], writes=["cqn"])
        s.dma("sp", ckvn[:], fm_view(ckvn_d), reads=["ckvn_d"], writes=["ckvn"])
        s.dma("sp", krT[:], kr_d, reads=["kr_d"], writes=["krT"])
        s.dma("sp", rC[:], g["ropeC_e"][:, 0:TOWN], writes=["rCq"])
        s.dma("sp", rS[:], g["ropeS_e"][:, 0:TOWN], writes=["rSq"])
        wq = [bd.sb(st, f"wq{i}", (128, 4, 256), BF16) for i in range(2)]
        wkv = [bd.sb(st, f"wkv{i}", (128, 4, 256), BF16) for i in range(2)]
        qT = bd.sb(st, "qT", (128, TOWN), BF16)
        qrT = bd.sb(st, "qrT", (64, TOWN), BF16)
        kT = bd.sb(st, "kT", (128, TS), BF16)
        V = bd.sb(st, "V", (128, NT_ALL, 128), BF16)
        pT = [bd.sb(st, f"pT{i}", (128, 512), BF16) for i in range(3)]
        tq = [bd.sb(st, f"tq{i}", (64, 512), F32) for i in range(2)]
        rec = bd.sb(st, "rec", (128, 512), F32)
        ast = [bd.sb(st, f"ast{i}", (128, TOWN), BF16) for i in range(2)]
        wuk, wkk = "w_uq_%d_all" % l, "w_ukv_%d_all" % l
        pn = 0
        for h in range(HEADS):
            wqt, wkvt = wq[h % 2], wkv[h % 2]
            qk_, kvk_ = ("wq", h % 2), ("wkv", h % 2)
            b0 = h * 192
            for (d0, s0, n) in ((0, b0, 192), (192, b0 + 160, 32), (224, b0 + 128, 32)):
                bd.wload(wqt[:, :, d0:d0 + n], W_uq[:, s0:s0 + n], qk_, wuk)
            bd.wload(wkvt[:], W_ukv[:, h * 256:(h + 1) * 256], kvk_, wkk)
            for (t0, tn) in own_blocks:
                ps, pk = bd.next_ps()
                s.mm(ps[:, 0:tn], [(wqt[:, kc, 0:128], cqn[:, kc, t0:t0 + tn]) for kc in range(4)], reads=[qk_, "cqn"], writes=[pk])
                s.op("act", lambda ps=ps, t0=t0, tn=tn: nc.scalar.copy(qT[:, t0:t0 + tn], ps[:, 0:tn]), reads=[pk], writes=["qT"])
                p1, k1 = bd.next_ps()
                p2, k2 = bd.next_ps()
                s.mm(p1[0:64, 0:tn], [(wqt[:, kc, 128:192], cqn[:, kc, t0:t0 + tn]) for kc in range(4)], reads=[qk_, "cqn"], writes=[k1])
                s.mm(p2[0:64, 0:tn], [(wqt[:, kc, 192:256], cqn[:, kc, t0:t0 + tn]) for kc in range(4)], reads=[qk_, "cqn"], writes=[k2])
                s.op("dve", lambda p1=p1, t0=t0, tn=tn: nc.vector.tensor_tensor(tq[0][:, 0:tn], p1[0:64, 0:tn], rC[:, t0:t0 + tn], ALU.mult), reads=[k1, "rCq"], writes=["tq0"])
                s.op("dve", lambda p2=p2, t0=t0, tn=tn: nc.vector.tensor_tensor(tq[1][:, 0:tn], p2[0:64, 0:tn], rS[:, t0:t0 + tn], ALU.mult), reads=[k2, "rSq"], writes=["tq1"])
                s.op("dve", lambda t0=t0, tn=tn: nc.vector.tensor_tensor(qrT[:, t0:t0 + tn], tq[0][:, 0:tn], tq[1][:, 0:tn], ALU.add), reads=["tq0", "tq1"], writes=["qrT"])
            for (t0, tn) in bd.tok_blocks(0, TS):
                ps, pk = bd.next_ps()
                s.mm(ps[:, 0:tn], [(wkvt[:, kc, 0:128], ckvn[:, kc, t0:t0 + tn]) for kc in range(4)], reads=[kvk_, "ckvn"], writes=[pk])
                s.op("act", lambda ps=ps, t0=t0, tn=tn: nc.scalar.copy(kT[:, t0:t0 + tn], ps[:, 0:tn]), reads=[pk], writes=["kT"])
            for t4 in range(0, NT_ALL, 4):
                ps, pk = bd.next_ps()
                nt = min(4, NT_ALL - t4)
                for j in range(nt):
                    t = t4 + j
                    s.mm(ps[:, j * 128:(j + 1) * 128], [(ckvn[:, kc, t * 128:(t + 1) * 128], wkvt[:, kc, 128:256]) for kc in range(4)],
                         reads=[kvk_, "ckvn"], writes=[pk])
                s.op("dve", lambda ps=ps, t4=t4, nt=nt: nc.vector.tensor_copy(V[:, t4:t4 + nt, :].rearrange("p t d -> p (t d)"), ps[:, 0:nt * 128]),
                     reads=[pk], writes=["V"])
            ab = ast[h % 2]
            ak = ("ast", h % 2)
            for qi, (q0, qn, ktiles) in enumerate(((0, 256, range(0, 2)), (256, 512, range(0, NT_ALL)), (768, 512, range(0, NT_ALL)))):
                bd.ps_n = 3
                bsel = 3 + 2 * ((h * 3 + qi) % 2)
                po, ko = bd.ps[bsel], ("ps", bsel)
                pz, kz = bd.ps[bsel + 1], ("ps", bsel + 1)
                kl = list(ktiles)
                for i, kt in enumerate(kl):
                    psc, ksc = bd.next_ps()
                    s.mm(psc[:, 0:qn], [(kT[:, kt * 128:(kt + 1) * 128], qT[:, q0:q0 + qn]), (krT[:, kt * 128:(kt + 1) * 128], qrT[:, q0:q0 + qn])],
                         reads=["kT", "qT", "krT", "qrT"], writes=[ksc])
                    pb = pT[pn % 3]
                    pkk = ("pT", pn % 3)
                    pn += 1
                    s.op("act", lambda psc=psc, pb=pb, qn=qn: nc.scalar.activation(pb[:, 0:qn], psc[:, 0:qn], AF.Exp, scale=ATT_SCALE),
                         reads=[ksc], writes=[pkk])
                    s.mm(po[:, 0:qn], [(V[:, kt, :], pb[:, 0:qn])], reads=["V", pkk], writes=[ko], start=(i == 0), stop=(i == len(kl) - 1))
                    s.mm(pz[:, 0:qn], [(ones_b[:], pb[:, 0:qn])], reads=["ones_b", pkk], writes=[kz], start=(i == 0), stop=(i == len(kl) - 1))
                s.op("dve", lambda pz=pz, qn=qn: nc.vector.reciprocal(rec[:, 0:qn], pz[:, 0:qn]), reads=[kz], writes=["rec"])
                s.op("dve", lambda po=po, q0=q0, qn=qn, ab=ab: nc.vector.tensor_tensor(ab[:, q0:q0 + qn], po[:, 0:qn], rec[:, 0:qn], ALU.mult),
                     reads=[ko, "rec"], writes=[ak])
            bd.ps_n = 7
            s.dma("sp", afm[h], ab[:], reads=[ak], writes=["afm"])
        s.barrier()
    if bd.stop_after == ("att", l):
        return
    layer_part3(bd, l, last, env, loc)


def layer_part3(bd, l, last, env, loc):
    nc, s = bd.nc, bd.s
    g = env
    Wg = g["Wg"]
    xcur, xmid, hfm, mfm, h2fm = g["xcur"], g["xmid"], g["hfm"], g["mfm"], g["h2fm"]
    ident_b, ident_f, ones_b, ones_f = g["ident_b"], g["ident_f"], g["ones_b"], g["ones_f"]
    modpp, psT = g["modpp"], g["psT"]
    convfm, fourfm, afm = loc["convfm"], loc["fourfm"], loc["afm"]
    gemm_fm, gemm_tm, norm_T, make_AB, own_blocks = loc["gemm_fm"], loc["gemm_tm"], loc["norm_T"], loc["make_AB"], loc["own_blocks"]
    NT_OWN = TOWN // 128
    W_in = Wg[("w_in", l)]

    with ExitStack() as st:
        hT = bd.sb(st, "hTo", (128, KC, TOWN), BF16)
        aT = bd.sb(st, "aT", (128, KC, TOWN), BF16)
        cT_ = bd.sb(st, "cvT", (128, 8, TOWN), BF16)
        fT = bd.sb(st, "fT", (128, 8, TOWN), BF16)
        s.dma("sp", hT[:], fm_view(hfm), reads=["hfm"], writes=["hTo"])
        s.dma("sp", aT[:], fm_view(afm), reads=["afm"], writes=["aT"])
        s.dma("sp", cT_[:], fm_view(convfm), reads=["convfm"], writes=["cvT"])
        s.dma("sp", fT[:], fm_view(fourfm), reads=["fourfm"], writes=["fT"])
        wm = [bd.sb(st, f"wm{i}", (128, 80, 128), BF16) for i in range(2)]
        sgm = [bd.sb(st, f"sgm{i}", (128, 512), F32) for i in range(3)]
        mt = [bd.sb(st, f"mt{i}", (128, 512), F32) for i in range(2)]
        mst = [bd.sb(st, "mst0", (128, TOWN), BF16)] * 2
        srcs = [(Wg[("w_mla_out", l)], 0, 16, "w_mla_out_%d_all" % l), (Wg[("w_conv_out", l)], 0, 8, "w_conv_out_%d_all" % l),
                (Wg[("w_four_out", l)], 0, 8, "w_four_out_%d_all" % l),
                (W_in, O_G, 16, "w_in_%d_all" % l), (W_in, O_G + D, 16, "w_in_%d_all" % l), (W_in, O_G + 2 * D, 16, "w_in_%d_all" % l)]
        koff = [0, 16, 24, 32, 48, 64]
        xin = [(aT, "aT"), (cT_, "cvT"), (fT, "fT"), (hT, "hTo"), (hT, "hTo"), (hT, "hTo")]

        def loadj(j):
            wt = wm[j % 2]
            for i, (W, cbase, kcn, wkey) in enumerate(srcs):
                bd.wload(wt[:, koff[i]:koff[i] + kcn, :], W[:, cbase + j * 128:cbase + (j + 1) * 128], ("wm", j % 2), wkey)
        loadj(0)
        for j in range(KC):
            if j + 1 < KC:
                loadj(j + 1)
            wt = wm[j % 2]
            wk = ("wm", j % 2)
            mb = mst[j % 2]
            mk = ("mst", 0)
            for (t0, tn) in own_blocks:
                pss = []
                for i in range(6):
                    ps, pk = bd.next_ps()
                    X, xk = xin[i]
                    kcn = srcs[i][2]
                    s.mm(ps[:, 0:tn], [(wt[:, koff[i] + kc, :], X[:, kc, t0:t0 + tn]) for kc in range(kcn)], reads=[wk, xk], writes=[pk])
                    pss.append((ps, pk))
                for i in range(3):
                    s.op("act", lambda i=i, tn=tn: nc.scalar.activation(sgm[i][:, 0:tn], pss[3 + i][0][:, 0:tn], AF.Sigmoid),
                         reads=[pss[3 + i][1]], writes=[("sgm", i)])
                s.op("dve", lambda tn=tn: nc.vector.tensor_tensor(mt[0][:, 0:tn], pss[0][0][:, 0:tn], sgm[0][:, 0:tn], ALU.mult),
                     reads=[pss[0][1], ("sgm", 0)], writes=["mt0"])
                for i in (1, 2):
                    s.op("dve", lambda i=i, tn=tn: nc.vector.tensor_tensor(mt[1][:, 0:tn], pss[i][0][:, 0:tn], sgm[i][:, 0:tn], ALU.mult),
                         reads=[pss[i][1], ("sgm", i)], writes=["mt1"])
                    if i == 1:
                        s.op("dve", lambda tn=tn: nc.vector.tensor_tensor(mt[0][:, 0:tn], mt[0][:, 0:tn], mt[1][:, 0:tn], ALU.add),
                             reads=["mt0", "mt1"], writes=["mt0"])
                    else:
                        s.op("dve", lambda tn=tn, t0=t0, mb=mb: nc.vector.tensor_tensor(mb[:, t0:t0 + tn], mt[0][:, 0:tn], mt[1][:, 0:tn], ALU.add),
                             reads=["mt0", "mt1"], writes=[mk])
            s.dma("sp", mfm[j], mb[:], reads=[mk], writes=["mfm"])
        s.barrier()

    with ExitStack() as st:
        mT = bd.sb(st, "mT", (128, KC, TOWN), BF16)
        s.dma("sp", mT[:], fm_view(mfm), reads=["mfm"], writes=["mT"])
        w16 = [bd.sb(st, f"w16_{i}", (128, KC, 512), BF16) for i in range(2)]
        xo = bd.sb(st, "xo", (128, NT_OWN, D), F32)
        s.dma("sp", xo[:], xcur[0:TOWN, :].rearrange("(t p) d -> p t d", p=128), reads=["xcur"], writes=["xo"])
        tmp = [bd.sb(st, f"rt{i}", (128, 512), F32) for i in range(2)]
        cn = [0]
        g_bc = bd.sb(st, "g_bc", (128, 2, D), F32)
        for v in range(2):
            s.dma("sp", g_bc[:, v, :], bcast_rows(loc["modsel"][v:v + 1, 2 * 2048:3 * 2048]), reads=["modsel"], writes=["g_bc"])

        def epi_o(gi, t, ps, pk, c0, n):
            tb = tmp[cn[0] % 2]
            tk = ("rt", cn[0] % 2)
            cn[0] += 1
            v = 1 if t < 2 else 0
            s.op("dve", lambda: nc.vector.tensor_tensor(tb[:, 0:n], ps[:, 0:n], g_bc[:, v, c0:c0 + n], ALU.mult), reads=[pk, "g_bc"], writes=[tk])
            s.op("dve", lambda: nc.vector.tensor_tensor(xo[:, t, c0:c0 + n], xo[:, t, c0:c0 + n], tb[:, 0:n], ALU.add), reads=[tk, ("xo", t)], writes=[("xo", t)])
        gemm_tm(Wg[("w_out", l)], 0, D, KC, mT, "mT", range(NT_OWN), w16, "w16", "w_out_%d_all" % l, epi_o)
        s.dma("sp", xmid.rearrange("(t p) d -> p t d", p=128), xo[:], reads=["xo"], writes=["xmid"])
        s.barrier()
    if bd.stop_after == ("mixer", l):
        return

    gates_d = bd.dram(f"gates_d{l}", (TOWN, NE))
    gatesT_d = bd.dram(f"gatesT_d{l}", (NE, TOWN))
    with ExitStack() as st:
        h2 = bd.sb(st, "h2", (128, KC, TOWN), BF16)
        AB = make_AB(st, g["norm2_e"], 3, 4)
        xt = [bd.sb(st, f"xt{i}", (128, D), F32) for i in range(2)]
        sq = bd.sb(st, "sq", (128, D), F32)
        ss = bd.sb(st, "ss", (128, 4), F32)
        xf = bd.sb(st, "xf", (128, D), F32)
        of32 = bd.sb(st, "of32", (128, KC, 128), F32)
        wr = bd.sb(st, "wr", (128, KC, NE), F32)
        brb = bd.sb(st, "brb", (128, NE), F32)
        lg = bd.sb(st, "lg", (128, NE), F32)
        m8 = bd.sb(st, "m8", (128, 8), F32)
        ex = bd.sb(st, "ex", (128, NE), F32)
        msk = bd.sb(st, "msk", (128, NE), F32)
        sm = bd.sb(st, "sm", (128, 2), F32)
        gt = bd.sb(st, "gt", (128, NT_OWN, NE), F32)
        gtT = bd.sb(st, "gtT", (NE, TOWN), F32)
        s.dma("sp", wr[:].rearrange("p k e -> p (k e)"), g["wrt_e"][l], writes=["wr"])
        s.dma("sp", brb[:], g["brt_e"][l], writes=["brb"])
        for t in range(NT_OWN):
            xb = xt[t % 2]
            s.dma("sp", xb[:], xmid[t * 128:(t + 1) * 128, :], reads=["xmid"], writes=[("xt", t % 2)])
            v = 1 if t < 2 else 0
            norm_T(st, xb[:], ("xt", t % 2), D, h2, "h2", t * 128, AB[:, v, 0, :], AB[:, v, 1, :], (sq, ss, None), fp32_out=(xf, of32))
            ps, pk = bd.next_ps()
            s.mm(ps[:, 0:NE], [(of32[:, kc, :], wr[:, kc, :]) for kc in range(KC)], reads=["of32", "wr"], writes=[pk])
            s.op("dve", lambda ps=ps: nc.vector.tensor_tensor(lg[:], ps[:, 0:NE], brb[:], ALU.add), reads=[pk, "brb"], writes=["lg"])
            s.op("dve", lambda: nc.vector.max(m8[:], lg[:]), reads=["lg"], writes=["m8"])
            s.op("dve", lambda: nc.vector.tensor_scalar(msk[:], lg[:], m8[:, 3:4], None, ALU.is_ge), reads=["lg", "m8"], writes=["msk"])
            s.op("dve", lambda: nc.vector.tensor_scalar(sm[:, 0:1], m8[:, 0:1], -1.0, None, ALU.mult), reads=["m8"], writes=["sm"])
            s.op("act", lambda: nc.scalar.activation(ex[:], lg[:], AF.Exp, bias=sm[:, 0:1], scale=1.0), reads=["lg", "sm"], writes=["ex"])
            s.op("dve", lambda: nc.vector.tensor_tensor(ex[:], ex[:], msk[:], ALU.mult), reads=["ex", "msk"], writes=["ex"])
            s.op("dve", lambda: nc.vector.reduce_sum(sm[:, 1:2], ex[:], axis=AX.X), reads=["ex", "sm"], writes=["sm"])
            s.op("dve", lambda: nc.vector.reciprocal(sm[:, 1:2], sm[:, 1:2]), reads=["sm"], writes=["sm"])
            s.op("dve", lambda t=t: nc.vector.tensor_scalar(gt[:, t, :], ex[:], sm[:, 1:2], None, ALU.mult), reads=["ex", "sm"], writes=["gt"])
            ps2, pk2 = bd.next_ps()
            s.transpose(ps2[0:NE, 0:128], gt[:, t, :], ident_f[:], reads=["gt", "ident_f"], writes=[pk2])
            s.op("dve", lambda ps2=ps2, t=t: nc.vector.tensor_copy(gtT[:, t * 128:(t + 1) * 128], ps2[0:NE, 0:128]), reads=[pk2], writes=["gtT"])
        s.dma("sp", fm_view(h2fm), h2[:], reads=["h2"], writes=["h2fm"])
        s.dma("sp", gates_d.rearrange("(t p) e -> p t e", p=128), gt[:], reads=["gt"], writes=["gates_d"])
        s.dma("sp", gatesT_d, gtT[:], reads=["gtT"], writes=["gatesT_d"])
        s.barrier()

    NPASS = 2
    PT = TOWN // NPASS
    for ps_i in range(NPASS):
        p0 = ps_i * PT
        blocks = bd.tok_blocks(p0, p0 + PT)
        ptiles = range(p0 // 128, (p0 + PT) // 128)
        with ExitStack() as st:
            h2 = bd.sb(st, "h2p", (128, KC, PT), BF16)
            s.dma("sp", h2[:], fm_view(h2fm)[:, :, p0:p0 + PT], reads=["h2fm"], writes=["h2p"])
            gt = bd.sb(st, "gtp", (128, PT // 128, NE), F32)
            s.dma("sp", gt[:], gates_d[p0:p0 + PT, :].rearrange("(t p) e -> p t e", p=128), reads=["gates_d"], writes=["gtp"])
            gtT = bd.sb(st, "gtTp", (NE, PT), F32)
            s.dma("sp", gtT[:], gatesT_d[:, p0:p0 + PT], reads=["gatesT_d"], writes=["gtTp"])
            bdn = bd.sb(st, "bdn", (NE, D), F32)
            s.dma("sp", bdn[:], g["bdn_e"][l], writes=["bdn"])
            bgu = bd.sb(st, "bgu", (128, NE, 16), F32)
            s.dma("sp", bgu[:].rearrange("p e c -> p (e c)"), g["bgu_e"][l], writes=["bgu"])
            yac = bd.sb(st, "yac", (128, PT // 128, D), F32)
            st_in = ExitStack()
            act = [bd.sb(st_in, f"actm{i}", (128, 8, PT), BF16) for i in range(1)] * 2
            wgu = [bd.sb(st_in, f"wgu{i}", (128, KC, 512), BF16) for i in range(3)]
            wdn = [bd.sb(st_in, f"wdn{i}", (128, 8, 512), BF16) for i in range(2)]
            tg = [bd.sb(st_in, f"tg{i}", (128, 512), F32) for i in range(1)] * 2
            tl = [bd.sb(st_in, f"tl{i}", (128, 512), F32) for i in range(1)] * 2
            tsg = [bd.sb(st_in, f"tsg{i}", (128, 512), F32) for i in range(1)] * 2
            for ti in range(PT // 128):
                for cb4 in range(4):
                    ps, pk = bd.next_ps()
                    s.mm(ps[:, :], [(gtT[:, ti * 128:(ti + 1) * 128], bdn[:, cb4 * 512:(cb4 + 1) * 512])], reads=["gtTp", "bdn"], writes=[pk])
                    s.op("act", lambda ps=ps, ti=ti, cb4=cb4: nc.scalar.copy(yac[:, ti, cb4 * 512:(cb4 + 1) * 512], ps[:, :]), reads=[pk], writes=[("yac", ti)])
            cnt = [0]
            for j in range(4):
                for r in range(NCORES):
                    e = 4 * r + j
                    Wgu = Wg[("gu", l, j)][r * D:(r + 1) * D, :]
                    Wdn = Wg[("dn", l, j)][r * DE:(r + 1) * DE, :]
                    ab = act[0]
                    ak = ("actm", 0)
                    for mg in range(4):
                        slot = bd.wslot["wgu"] % 3
                        bd.wslot["wgu"] += 1
                        wt = wgu[slot]
                        wk = ("wgu", slot)
                        bd.wload(wt[:, :, 0:256], Wgu[:, mg * 256:(mg + 1) * 256], wk, "w_gu_%d_%d_all" % (l, j))
                        bd.wload(wt[:, :, 256:512], Wgu[:, DE + mg * 256:DE + (mg + 1) * 256], wk, "w_gu_%d_%d_all" % (l, j))
                        for mm_ in range(2):
                            m = mg * 2 + mm_
                            for (t0, tn) in blocks:
                                pg, kg = bd.next_ps()
                                pl, kl = bd.next_ps()
                                s.mm(pg[:, 0:tn], [(wt[:, kc, mm_ * 128:(mm_ + 1) * 128], h2[:, kc, t0 - p0:t0 - p0 + tn]) for kc in range(KC)], reads=[wk, "h2p"], writes=[kg])
                                s.mm(pl[:, 0:tn], [(wt[:, kc, 256 + mm_ * 128:256 + (mm_ + 1) * 128], h2[:, kc, t0 - p0:t0 - p0 + tn]) for kc in range(KC)], reads=[wk, "h2p"], writes=[kl])
                                i2 = 0
                                a_, b_, c_ = tg[i2], tl[i2], tsg[i2]
                                s.op("dve", lambda pg=pg, a_=a_, tn=tn, e=e, m=m: nc.vector.tensor_scalar(a_[:, 0:tn], pg[:, 0:tn], bgu[:, e, m:m + 1], 7.0, ALU.add, ALU.min),
                                     reads=[kg, "bgu"], writes=[("tg", i2)])
                                s.op("act", lambda a_=a_, c_=c_, tn=tn: nc.scalar.activation(c_[:, 0:tn], a_[:, 0:tn], AF.Sigmoid, scale=1.702),
                                     reads=[("tg", i2)], writes=[("tsg", i2)])
                                s.op("dve", lambda pl=pl, b_=b_, tn=tn, e=e, m=m: nc.vector.tensor_scalar(b_[:, 0:tn], pl[:, 0:tn], bgu[:, e, 8 + m:9 + m], 7.0, ALU.add, ALU.min),
                                     reads=[kl, "bgu"], writes=[("tl", i2)])
                                s.op("dve", lambda b_=b_, tn=tn: nc.vector.tensor_scalar(b_[:, 0:tn], b_[:, 0:tn], -7.0, 1.0, ALU.max, ALU.add),
                                     reads=[("tl", i2)], writes=[("tl", i2)])
                                s.op("dve", lambda a_=a_, c_=c_, tn=tn: nc.vector.tensor_tensor(a_[:, 0:tn], a_[:, 0:tn], c_[:, 0:tn], ALU.mult),
                                     reads=[("tg", i2), ("tsg", i2)], writes=[("tg", i2)])
                                s.op("dve", lambda a_=a_, b_=b_, tn=tn, t0=t0, m=m, ab=ab: nc.vector.tensor_tensor(ab[:, m, t0 - p0:t0 - p0 + tn], a_[:, 0:tn], b_[:, 0:tn], ALU.mult),
                                     reads=[("tg", i2), ("tl", i2)], writes=[ak])
                    for cb4 in range(4):
                        slot = bd.wslot["wdn"] % 2
                        bd.wslot["wdn"] += 1
                        wt = wdn[slot]
                        wk = ("wdn", slot)
                        bd.wload(wt[:], Wdn[:, cb4 * 512:(cb4 + 1) * 512], wk, "w_dn_%d_%d_all" % (l, j))
                        for ti in range(PT // 128):
                            ps, pk = bd.next_ps()
                            s.mm(ps[:, :], [(ab[:, kc, ti * 128:(ti + 1) * 128], wt[:, kc, :]) for kc in range(8)], reads=[ak, wk], writes=[pk])
                            s.op("dve", lambda ps=ps, ti=ti, cb4=cb4, e=e: nc.vector.scalar_tensor_tensor(
                                yac[:, ti, cb4 * 512:(cb4 + 1) * 512], ps[:, :], gt[:, ti, e:e + 1], yac[:, ti, cb4 * 512:(cb4 + 1) * 512], ALU.mult, ALU.add),
                                reads=[pk, "gtp", ("yac", ti)], writes=[("yac", ti)])
            s.barrier()
            st_in.close()
            g_bc = bd.sb(st, "g_bc", (128, 2, D), F32)
            for v in range(2):
                s.dma("sp", g_bc[:, v, :], bcast_rows(loc["modsel"][v:v + 1, 5 * 2048:6 * 2048]), reads=["modsel"], writes=["g_bc"])
            nfin_sb = bd.sb(st, "nfin_sb", (128, D), F32)
            s.dma("sp", nfin_sb[:], g["nfin_e"], writes=["nfin"])
            tl = [bd.sb(st, "tlr", (128, 8), F32)] * 2
            xr = [bd.sb(st, f"xr{i}", (128, D), F32) for i in range(1)] * 2
            for ti in range(PT // 128):
                t = p0 // 128 + ti
                xb = xr[ti % 2]
                xk = ("xr", 0)
                v = 1 if t < 2 else 0
                s.dma("sp", xb[:], xmid[t * 128:(t + 1) * 128, :], reads=["xmid"], writes=[xk])
                s.op("dve", lambda ti=ti, v=v: nc.vector.tensor_tensor(yac[:, ti, :], yac[:, ti, :], g_bc[:, v, :], ALU.mult), reads=[("yac", ti), "g_bc"], writes=[("yac", ti)])
                s.op("dve", lambda ti=ti, xb=xb: nc.vector.tensor_tensor(xb[:], xb[:], yac[:, ti, :], ALU.add), reads=[("yac", ti), xk], writes=[xk])
                if not last:
                    s.dma("sp", xcur[t * 128:(t + 1) * 128, :], xb[:], reads=[xk], writes=["xcur"])
                    if t >= 2:
                        s.dma("sp", g["xown"][(t - 2) * 128:(t - 1) * 128, :], xb[:], reads=[xk], writes=["xown"])
                elif t >= 2:
                    s.op("act", lambda xb=xb: nc.scalar.activation(yac[:, ti, :], xb[:], AF.Square), reads=[xk, ("yac", ti)], writes=[("yac", ti)])
                    s.op("dve", lambda ti=ti: nc.vector.reduce_sum(tl[0][:, 0:1], yac[:, ti, :], axis=AX.X), reads=[("yac", ti)], writes=[("tl", 0)])
                    s.op("dve", lambda: nc.vector.tensor_scalar(tl[0][:, 1:2], tl[0][:, 0:1], 1.0 / D, EPS, ALU.mult, ALU.add), reads=[("tl", 0)], writes=[("tl", 0)])
                    s.op("act", lambda: nc.scalar.activation(tl[0][:, 2:3], tl[0][:, 1:2], AF.Sqrt), reads=[("tl", 0)], writes=[("tl", 0)])
                    s.op("dve", lambda: nc.vector.reciprocal(tl[0][:, 3:4], tl[0][:, 2:3]), reads=[("tl", 0)], writes=[("tl", 0)])
                    s.op("dve", lambda xb=xb: nc.vector.tensor_scalar(xb[:], xb[:], tl[0][:, 3:4], None, ALU.mult), reads=[xk, ("tl", 0)], writes=[xk])
                    s.op("dve", lambda xb=xb: nc.vector.tensor_tensor(xb[:], xb[:], nfin_sb[:], ALU.mult), reads=[xk, "nfin"], writes=[xk])
                    s.dma("sp", g["out_e"][(t - 2) * 128:(t - 1) * 128, :], xb[:], reads=[xk], writes=["out"])
            s.barrier()

    if not last:
        s.allgather(g["xown"], g["xgath"], reads=["xown"], writes=["xgath"])
        with ExitStack() as st:
            sel8 = bd.sb(st, "sel8", (128, 8), F32)
            s.dma("sp", sel8[:], g["sel8_e"], writes=["sel8"])
            xa = bd.sb(st, "xa", (128, D), F32)
            xb2 = [bd.sb(st, f"xg{i}", (128, D), F32) for i in range(2)]
            n = 0
            for t in range(HALF // 128):
                for r in range(NCORES):
                    xb = xb2[n % 2]
                    xk = ("xg", n % 2)
                    n += 1
                    s.dma("sp", xb[:], g["xgath"][r * HALF + t * 128:r * HALF + (t + 1) * 128, :], reads=["xgath", xk], writes=[xk])
                    if r == 0:
                        s.op("dve", lambda xb=xb, r=r: nc.vector.tensor_scalar(xa[:], xb[:], sel8[:, r:r + 1], None, ALU.mult), reads=[xk, "sel8", "xa"], writes=["xa"])
                    else:
                        s.op("dve", lambda xb=xb, r=r: nc.vector.scalar_tensor_tensor(xa[:], xb[:], sel8[:, r:r + 1], xa[:], ALU.mult, ALU.add), reads=[xk, "sel8", "xa"], writes=["xa"])
                s.dma("sp", xcur[TOWN + t * 128:TOWN + (t + 1) * 128, :], xa[:], reads=["xa"], writes=["xcur"])
            s.barrier()


def _pp(v, nch):
    return np.ascontiguousarray(v.reshape(nch, 128).T)


_CACHE = {}


def _consts():
    if "c" in _CACHE:
        return _CACHE["c"]
    bf = ml_dtypes.bfloat16
    c = {}
    c["ident_bf"] = np.eye(128, dtype=np.float32).astype(bf)
    c["ident_f32"] = np.eye(128, dtype=np.float32)
    t = np.arange(SEQ)
    row, col = (t // 64).astype(np.float64), (t % 64).astype(np.float64)
    inv = 10000.0 ** (-np.arange(0, 32, 2, dtype=np.float64) / 32)
    ang = np.concatenate([row[:, None] * inv, col[:, None] * inv], axis=-1)
    cos, sin = np.cos(ang).astype(np.float32), np.sin(ang).astype(np.float32)
    c["cos"], c["sin"] = cos, sin
    m = np.arange(256, dtype=np.float64)
    angc = 2 * np.pi * np.outer(m, m) / 256
    c["chC"] = (np.cos(angc) / 16).astype(np.float32).astype(bf)
    c["chSn"] = (-np.sin(angc) / 16).astype(np.float32).astype(bf)
    c["cposC"] = (np.cos(angc) / 16).astype(np.float32).astype(bf)
    c["cposS"] = (np.sin(angc) / 16).astype(np.float32).astype(bf)
    n = np.arange(SEQ, dtype=np.float64)
    angp = 2 * np.pi * np.outer(n, n) / SEQ
    c["posC"] = (np.cos(angp) / np.sqrt(SEQ)).astype(np.float32)
    c["posS"] = (np.sin(angp) / np.sqrt(SEQ)).astype(np.float32)
    _CACHE["c"] = c
    return c


def _core_inputs(core, inp, bd):
    bf = ml_dtypes.bfloat16
    c = _consts()
    b, h = core // 2, core % 2
    own = slice(h * HALF, (h + 1) * HALF)
    oth = slice((1 - h) * HALF, (2 - h) * HALF)
    m = {}
    m["x_loc"] = np.concatenate([inp["ctx"][b], inp["x"][b][own], inp["x"][b][oth]], axis=0)
    cv = np.concatenate([inp["c"], inp["c_ctx"][None, :]], axis=0)
    m["cT"] = np.ascontiguousarray(cv.reshape(5, KC, 128).transpose(2, 1, 0)).reshape(128, KC * 5)
    oh = np.zeros((128, 4), np.float32); oh[:, b] = 1.0
    m["onehot_b"] = oh
    s8 = np.zeros((128, 8), np.float32); s8[:, core ^ 1] = 1.0
    m["sel8"] = s8
    order = np.concatenate([np.arange(SEQ)[own], np.arange(SEQ)[oth]])
    cosl, sinl = c["cos"][order], c["sin"][order]
    rc = np.ones((64, TS), np.float32); rs = np.zeros((64, TS), np.float32)
    rc[0:32, CTX:] = cosl.T; rc[32:64, CTX:] = cosl.T
    rs[0:32, CTX:] = -sinl.T; rs[32:64, CTX:] = sinl.T
    m["ropeC"], m["ropeS"] = rc, rs
    hm = np.zeros((128, 2), np.float32); hm[:, 0] = float(h == 1); hm[:, 1] = float(h == 0)
    m["halo_mask"] = hm
    m["posC"] = np.ascontiguousarray(c["posC"][order][:, own]).astype(bf)
    m["posS"] = np.ascontiguousarray(c["posS"][order][:, own]).astype(bf)
    for k in ("cposC", "cposS", "chC", "chSn", "ident_bf", "ident_f32"):
        m[k] = c[k]
    m["norm1_pp"] = np.stack([_pp(inp["norm1"][l], KC) for l in range(L)])
    m["norm2_pp"] = np.stack([_pp(inp["norm2"][l], KC) for l in range(L)])
    m["qnorm_pp"] = np.stack([_pp(inp["q_norm"][l], 4) for l in range(L)])
    m["kvnorm_pp"] = np.stack([_pp(inp["kv_norm"][l], 4) for l in range(L)])
    m["conv_dw_pp"] = np.stack([np.ascontiguousarray(inp["conv_dw"][l].reshape(31, 8, 128).transpose(2, 1, 0)).reshape(128, 8 * 31) for l in range(L)])
    m["conv_b_pp"] = np.stack([_pp(inp["conv_dw_b"][l], 8) for l in range(L)])
    m["conv_lng_pp"] = np.stack([_pp(inp["conv_ln_g"][l], 8) for l in range(L)])
    m["conv_lnb_pp"] = np.stack([_pp(inp["conv_ln_b"][l], 8) for l in range(L)])
    m["b_gu_pp"] = np.stack([np.ascontiguousarray(inp["b_gate_up"][l].reshape(NE, 16, 128).transpose(2, 0, 1)).reshape(128, NE * 16) for l in range(L)])
    m["b_router_bc"] = np.stack([np.broadcast_to(inp["b_router"][l][None, :], (128, NE)).copy() for l in range(L)])
    m["b_down"] = np.ascontiguousarray(inp["b_down"])
    m["w_router_pp"] = np.stack([np.ascontiguousarray(inp["w_router"][l].reshape(KC, 128, NE).transpose(1, 0, 2)).reshape(128, KC * NE) for l in range(L)])
    m["norm_final_bc"] = np.broadcast_to(inp["norm_final"][None, :], (128, D)).copy()
    m["b_ada_sh"] = np.ascontiguousarray(inp["b_ada"][:, None, core * ADA_SH:(core + 1) * ADA_SH])
    m["w_ada_sh"] = np.ascontiguousarray(inp["w_ada"][:, :, core * ADA_SH:(core + 1) * ADA_SH])
    for l in range(bd.nlayers):
        for nm in ("w_in", "w_uq", "w_ukv", "w_mla_out", "w_conv_out", "w_four_out", "w_out"):
            W = inp[nm][l]
            r = W.shape[0] // 8
            m[f"{nm}_{l}"] = np.ascontiguousarray(W[core * r:(core + 1) * r])
        if bd.with_moe:
            for j in range(4):
                m[f"w_gu_{l}_{j}"] = np.ascontiguousarray(inp["w_gate_up"][l, 4 * core + j])
                m[f"w_dn_{l}_{j}"] = np.ascontiguousarray(inp["w_down"][l, 4 * core + j])
    out = {}
    for k, (shape, dt) in bd.inputs.items():
        a = m[k]
        assert tuple(a.shape) == tuple(shape), (k, a.shape, shape)
        out[k] = a
    return out


def kernel(**inputs):
    inp = {k: np.asarray(v) for k, v in inputs.items()}
    bd = Builder()
    bd.stop_after = None
    build_program(bd)
    in_maps = [_core_inputs(c, inp, bd) for c in range(NCORES)]
    res = run_bass_kernel_spmd(bd.nc, in_maps, core_ids=list(range(NCORES)))
    out = np.zeros((B, SEQ, D), np.float32)
    for c in range(NCORES):
        b, h = c // 2, c % 2
        out[b, h * HALF:(h + 1) * HALF] = res.results[c]["out"]
    return out
```

```python
import math
import numpy as np
import ml_dtypes
import concourse.bass as bass
import concourse.mybir as mybir
from concourse.bass_utils import run_bass_kernel_spmd

F32 = mybir.dt.float32
BF16 = mybir.dt.bfloat16
ALU = mybir.AluOpType
AF = mybir.ActivationFunctionType
AX = mybir.AxisListType

NCORES = 8
D = 2048
KC = 16
B = 4
SEQ = 2048
CTX = 256
TS = CTX + SEQ
HALF = 1024
TOWN = CTX + HALF
L = 2
NE = 32
DE = 1024
HEADS = 16
INC = 10304
O_CQ, O_CKV, O_KR, O_U, O_F, O_G = 0, 512, 1024, 1088, 3136, 4160
EPS = 1e-6
ATT_SCALE = 192 ** -0.5
ADA_SH = 12288 // 8


class Rec:
    __slots__ = ("w", "r")

    def __init__(self):
        self.w = None
        self.r = {}


class Sched:
    def __init__(self, nc):
        self.nc = nc
        self.eng = {"pe": nc.tensor, "act": nc.scalar, "dve": nc.vector, "pool": nc.gpsimd, "sp": nc.sync}
        self.sem = {k: nc.alloc_semaphore("s_" + k) for k in ["pe", "act", "dve", "pool"]}
        self.cnt = {k: 0 for k in self.sem}
        self.ND = 36
        self.dsem = [nc.alloc_semaphore(f"d{i}") for i in range(self.ND)]
        self.duse = [0] * self.ND
        self.dnext = 0
        self.csem = []
        self.waited = {k: {} for k in self.eng}
        self.state = {}
        self.nps = 0

    def _semof(self, k):
        if isinstance(k, tuple):
            return self.dsem[k[1]] if k[0] == "d" else self.csem[k[1]]
        return self.sem[k]

    def _norm(self, key):
        return key if isinstance(key, tuple) else (key, None)

    def _recs(self, key):
        name, idx = self._norm(key)
        d = self.state.setdefault(name, {})
        if idx is None:
            return list(d.values())
        out = []
        if idx in d:
            out.append(d[idx])
        if None in d:
            out.append(d[None])
        return out

    def _deps(self, reads, writes):
        toks = {}

        def add(t):
            if t is None:
                return
            k, v = t
            if toks.get(k, 0) < v:
                toks[k] = v
        for key in reads:
            for rec in self._recs(key):
                add(rec.w)
        for key in writes:
            for rec in self._recs(key):
                add(rec.w)
                for k, v in rec.r.items():
                    add((k, v))
        return toks

    def _record(self, reads, writes, tok):
        for key in reads:
            name, idx = self._norm(key)
            d = self.state.setdefault(name, {})
            rec = d.setdefault(idx, Rec())
            if rec.r.get(tok[0], 0) < tok[1]:
                rec.r[tok[0]] = tok[1]
        for key in writes:
            name, idx = self._norm(key)
            d = self.state.setdefault(name, {})
            if idx is None:
                d.clear()
            rec = Rec()
            rec.w = tok
            d[idx] = rec

    def _wait(self, E, toks):
        w = self.waited[E]
        for k, v in toks.items():
            if k == E and E == "pe":
                continue
            if w.get(k, 0) >= v:
                continue
            self.eng[E].wait_ge(self._semof(k), v)
            w[k] = v

    def op(self, E, fn, reads=(), writes=()):
        self._wait(E, self._deps(reads, writes))
        ins = fn()
        self.cnt[E] += 1
        ins.then_inc(self.sem[E], 1)
        self._record(reads, writes, (E, self.cnt[E]))
        return ins

    def mm(self, out, pairs, reads, writes, start=True, stop=True):
        self._wait("pe", self._deps(reads, writes))
        n = len(pairs)
        ins = None
        for i, (lt, rh) in enumerate(pairs):
            ins = self.nc.tensor.matmul(out, lt, rh, start=(start and i == 0), stop=(stop and i == n - 1))
        self.cnt["pe"] += 1
        ins.then_inc(self.sem["pe"], 1)
        self._record(reads, writes, ("pe", self.cnt["pe"]))

    def transpose(self, out, in_, ident, reads, writes):
        self._wait("pe", self._deps(reads, writes))
        ins = self.nc.tensor.transpose(out, in_, ident)
        self.cnt["pe"] += 1
        ins.then_inc(self.sem["pe"], 1)
        self._record(reads, writes, ("pe", self.cnt["pe"]))

    def dma(self, Q, out, in_, reads=(), writes=()):
        toks = self._deps(reads, writes)
        i = self.dnext
        self.dnext = (self.dnext + 1) % self.ND
        if self.duse[i] > 0:
            k = ("d", i)
            if toks.get(k, 0) < 16 * self.duse[i]:
                toks[k] = 16 * self.duse[i]
        self._wait(Q, toks)
        self.duse[i] += 1
        self.eng[Q].dma_start(out=out, in_=in_).then_inc(self.dsem[i], 16)
        self._record(reads, writes, (("d", i), 16 * self.duse[i]))

    def allgather(self, src, dst, reads, writes, qos=None):
        self._wait("pool", self._deps(reads, writes))
        i = len(self.csem)
        self.csem.append(self.nc.alloc_semaphore(f"c{i}"))
        self.nc.gpsimd.collective_compute(
            "AllGather", ALU.bypass, replica_groups=[list(range(NCORES))],
            ins=[src.opt()], outs=[dst.opt()], dma_qos=qos).then_inc(self.csem[i])
        self._record(reads, writes, (("c", i), 1))

    def barrier(self, engines=None):
        toks = {k: v for k, v in self.cnt.items() if v > 0}
        for i in range(self.ND):
            if self.duse[i]:
                toks[("d", i)] = 16 * self.duse[i]
        for E in (engines or self.eng):
            t = dict(toks)
            t.pop(E, None)
            if E == "pe":
                pass
            self._wait(E, t)
        if engines is None:
            keep = {}
            for name, d in self.state.items():
                for idx, rec in d.items():
                    if rec.w is not None and isinstance(rec.w[0], tuple) and rec.w[0][0] == "c":
                        r2 = Rec()
                        r2.w = rec.w
                        keep.setdefault(name, {})[idx] = r2
            self.state = keep


class Builder:
    def __init__(self, debug_stage=None, nlayers=L, with_moe=True):
        self.debug_stage = debug_stage
        self.nlayers = nlayers
        self.with_moe = with_moe
        self.nc = bass.Bass("TRN2", target_bir_lowering=False)
        self.s = Sched(self.nc)
        self.inputs = {}
        self.psn = 0

    def ext(self, name, shape, dt=F32):
        ap = self.nc.dram_tensor(name, list(shape), dt, kind="ExternalInput").ap()
        self.inputs[name] = (tuple(shape), dt)
        return ap

    def dram(self, name, shape, dt=F32):
        ap = self.nc.dram_tensor(name, list(shape), dt).ap()
        if not hasattr(self, "scratch"):
            self.scratch = {}
        self.scratch[name] = (ap, tuple(shape), dt)
        return ap

    def sb(self, stack, name, shape, dt):
        self.uid = getattr(self, "uid", 0) + 1
        return stack.enter_context(self.nc.sbuf_tensor(f"sb{self.uid}_{name}", list(shape), dt))

    def next_ps(self):
        n = getattr(self, "ps_n", 7)
        i = self.psn % n
        self.psn += 1
        return self.ps[i], ("ps", i)

    def gather_weight(self, name, shape):
        R, C = shape
        e = self.ext(name, shape)
        src = self.dram(name + "_src", shape, BF16)
        dst = self.dram(name + "_all", (NCORES * R, C), BF16)
        self.s.dma("pool", src, e, reads=[], writes=[name + "_src"])
        self.pending_ag.append((src, dst, name))
        return dst

    def flush_gathers(self, qos=None):
        for src, dst, name in self.pending_ag:
            self.s.allgather(src, dst, reads=[name + "_src"], writes=[name + "_all"], qos=qos)
        self.pending_ag = []

    def load_w(self, wt, W, c0, ncols, kc, key, wkey):
        self.wload(wt[:, 0:kc, 0:ncols], W[:, c0:c0 + ncols], key, wkey)

    def wload(self, dst, src2d, key, wkey):
        self.s.dma("sp", dst, src2d.rearrange("(kc p) n -> p kc n", p=128), reads=[wkey, key], writes=[key])

    def tok_blocks(self, t0, t1, bs=512):
        out = []
        t = t0
        while t < t1:
            n = min(bs, t1 - t)
            out.append((t, n))
            t += n
        return out


from contextlib import ExitStack


def build_program(bd, dbg=None):
    nc, s = bd.nc, bd.s
    bd.ps = [nc.alloc_psum_tensor(f"ps{i}", [128, 512], F32) for i in range(7)]
    psT = nc.alloc_psum_tensor("psT", [128, 1024], BF16)

    x_loc = bd.ext("x_loc", (TS, D))
    cT_e = bd.ext("cT", (128, KC * 5))
    onehot_e = bd.ext("onehot_b", (128, 4))
    sel8_e = bd.ext("sel8", (128, 8))
    ropeC_e = bd.ext("ropeC", (64, TS))
    ropeS_e = bd.ext("ropeS", (64, TS))
    halo_e = bd.ext("halo_mask", (128, 2))
    posC_e = bd.ext("posC", (SEQ, HALF), BF16)
    posS_e = bd.ext("posS", (SEQ, HALF), BF16)
    cposC_e = bd.ext("cposC", (CTX, CTX), BF16)
    cposS_e = bd.ext("cposS", (CTX, CTX), BF16)
    chC_e = bd.ext("chC", (256, 256), BF16)
    chSn_e = bd.ext("chSn", (256, 256), BF16)
    identb_e = bd.ext("ident_bf", (128, 128), BF16)
    identf_e = bd.ext("ident_f32", (128, 128))
    norm1_e = bd.ext("norm1_pp", (L, 128, KC))
    norm2_e = bd.ext("norm2_pp", (L, 128, KC))
    qn_e = bd.ext("qnorm_pp", (L, 128, 4))
    kvn_e = bd.ext("kvnorm_pp", (L, 128, 4))
    cdw_e = bd.ext("conv_dw_pp", (L, 128, 8 * 31))
    cdb_e = bd.ext("conv_b_pp", (L, 128, 8))
    clg_e = bd.ext("conv_lng_pp", (L, 128, 8))
    clb_e = bd.ext("conv_lnb_pp", (L, 128, 8))
    bgu_e = bd.ext("b_gu_pp", (L, 128, NE * 16))
    brt_e = bd.ext("b_router_bc", (L, 128, NE))
    bdn_e = bd.ext("b_down", (L, NE, D))
    wrt_e = bd.ext("w_router_pp", (L, 128, KC * NE))
    nfin_e = bd.ext("norm_final_bc", (128, D))
    bada_e = bd.ext("b_ada_sh", (L, 1, ADA_SH))
    wada_e = bd.ext("w_ada_sh", (L, D, ADA_SH))
    out_e = nc.dram_tensor("out", [HALF, D], F32, kind="ExternalOutput").ap()

    xcur = bd.dram("xcur", (TS, D))
    xmid = bd.dram("xmid", (TOWN, D))
    hfm = bd.dram("hfm", (KC, 128, TOWN), BF16)
    mfm = bd.dram("mfm", (KC, 128, TOWN), BF16)
    h2fm = bd.dram("h2fm", (KC, 128, TOWN), BF16)
    modpart = bd.dram("modpart", (12, 10 * 128))
    modall = bd.dram("modall", (96, 10 * 128))
    xown = bd.dram("xown", (HALF, D))
    xgath = bd.dram("xgath", (NCORES * HALF, D))

    with ExitStack() as gs:
        ident_b = bd.sb(gs, "ident_b", (128, 128), BF16)
        ident_f = bd.sb(gs, "ident_f", (128, 128), F32)
        ones_b = bd.sb(gs, "ones_b", (128, 128), BF16)
        ones_f = bd.sb(gs, "ones_f", (128, 128), F32)
        onehot = bd.sb(gs, "onehot", (128, 4), F32)
        halo_m = bd.sb(gs, "halo_m", (128, 2), F32)
        modpp = bd.sb(gs, "modpp", (128, 2, 6 * KC), F32)
        s.dma("sp", ident_b[:], identb_e, writes=["ident_b"])
        s.dma("sp", ident_f[:], identf_e, writes=["ident_f"])
        s.dma("sp", onehot[:], onehot_e, writes=["onehot"])
        s.dma("sp", halo_m[:], halo_e, writes=["halo_m"])
        s.op("dve", lambda: nc.vector.memset(ones_b[:], 1.0), writes=["ones_b"])
        s.op("dve", lambda: nc.vector.memset(ones_f[:], 1.0), writes=["ones_f"])
        s.dma("sp", xcur, x_loc, writes=["xcur"])

        with ExitStack() as st:
            cT = bd.sb(st, "cT", (128, KC, 5), F32)
            scT = bd.sb(st, "scT", (128, KC, 5), BF16)
            wad = [bd.sb(st, f"wad{i}", (128, KC, 512), BF16) for i in range(2)]
            bad = bd.sb(st, "bad", (1, ADA_SH), F32)
            msb = bd.sb(st, "msb", (5, ADA_SH), F32)
            s.dma("sp", cT[:].rearrange("p k f -> p (k f)"), cT_e, writes=["cT"])
            s.op("act", lambda: nc.scalar.activation(scT[:].rearrange("p k f -> p (k f)"),
                                                     cT[:].rearrange("p k f -> p (k f)"), AF.Silu),
                 reads=["cT"], writes=["scT"])
            n = 0
            for l in range(L):
                s.dma("sp", bad[:], bada_e[l], reads=["bad"], writes=["bad"])
                for blk in range(3):
                    wt = wad[n % 2]
                    wk = ("wad", n % 2)
                    n += 1
                    s.dma("pool", wt[:, 0:KC, 0:512], wada_e[l][:, blk * 512:(blk + 1) * 512].rearrange("(kc p) n -> p kc n", p=128),
                          reads=[wk], writes=[wk])
                    ps, pk = bd.next_ps()
                    pairs = [(scT[:, kc, :], wt[:, kc, :]) for kc in range(KC)]
                    pairs.append((ones_f[0:1, 0:5], bad[0:1, blk * 512:(blk + 1) * 512]))
                    s.mm(ps[0:5, :], pairs, reads=["scT", wk, "bad", "ones_f"], writes=[pk])
                    s.op("dve", lambda ps=ps, blk=blk: nc.vector.tensor_copy(msb[:, blk * 512:(blk + 1) * 512], ps[0:5, :]),
                         reads=[pk], writes=["msb"])
                dst = modpart.rearrange("cc (q p) -> q cc p", p=128)[l * 5:(l + 1) * 5]
                s.dma("sp", dst, msb[:].rearrange("r (cc p) -> r cc p", p=128), reads=["msb"], writes=["modpart"])
            s.allgather(modpart, modall, reads=["modpart"], writes=["modall"])
            s.barrier()

        Wg = {}
        bd.pending_ag = []
        wdefs = [("w_in", D // 8, INC), ("w_uq", 64, 3072), ("w_ukv", 64, 4096), ("w_mla_out", D // 8, D),
                 ("w_conv_out", 128, D), ("w_four_out", 128, D), ("w_out", D // 8, D)]
        for l in range(bd.nlayers):
            for nm, r, c in wdefs:
                Wg[(nm, l)] = bd.gather_weight(f"{nm}_{l}", (r, c))
            if bd.with_moe:
                for j in range(4):
                    Wg[("gu", l, j)] = bd.gather_weight(f"w_gu_{l}_{j}", (D, 2 * DE))
                    Wg[("dn", l, j)] = bd.gather_weight(f"w_dn_{l}_{j}", (DE, D))
        bd.flush_gathers(qos="P3")


        for l in range(bd.nlayers):
            last = (l == L - 1)
            layer(bd, l, last, locals())
            if dbg is not None and dbg[0] == l:
                break
        if dbg is not None:
            for nm in dbg[1]:
                ap, shape, dt = bd.scratch[nm]
                o = nc.dram_tensor("dbg_" + nm, list(shape), dt, kind="ExternalOutput").ap()
                s.dma("sp", o, ap, reads=[nm], writes=["dbg_" + nm])
            s.barrier()
    return nc


def fm_view(ap3):
    return ap3.rearrange("c p t -> p c t")


def bcast_rows(ap2, nparts=128):
    n = ap2.shape[-1]
    return bass.AP(ap2.tensor, ap2.offset, [[0, nparts], [1, n]])


def layer(bd, l, last, env):
    nc, s = bd.nc, bd.s
    g = env
    Wg = g["Wg"]
    xcur, xmid, hfm, mfm, h2fm, modall = g["xcur"], g["xmid"], g["hfm"], g["mfm"], g["h2fm"], g["modall"]
    ident_b, ident_f, ones_b, ones_f = g["ident_b"], g["ident_f"], g["ones_b"], g["ones_f"]
    onehot, halo_m, modpp, psT = g["onehot"], g["halo_m"], g["modpp"], g["psT"]
    NT_OWN = TOWN // 128
    NT_ALL = TS // 128
    T0 = CTX if last else 0
    own_blocks = bd.tok_blocks(T0, TOWN)

    cqn_d = bd.dram(f"cqn_d{l}", (4, 128, TOWN), BF16)
    ckvn_d = bd.dram(f"ckvn_d{l}", (4, 128, TS), BF16)
    kr_d = bd.dram(f"kr_d{l}", (64, TS), BF16)
    fz_d = bd.dram(f"fz_d{l}", (TS, 1024), BF16)
    convfm = bd.dram(f"convfm{l}", (8, 128, TOWN), BF16)
    fourfm = bd.dram(f"fourfm{l}", (8, 128, TOWN), BF16)
    afm = bd.dram(f"afm{l}", (16, 128, TOWN), BF16)
    modsel = bd.dram(f"modsel{l}", (2, 96 * 128))

    with ExitStack() as st:
        mrows = bd.sb(st, "mrows", (96, 5, 128), F32)
        msel = bd.sb(st, "msel", (96, 2, 128), F32)
        src = modall.rearrange("gc (q p) -> gc q p", p=128)[:, l * 5:(l + 1) * 5, :]
        s.dma("sp", mrows[:], src, reads=["modall"], writes=["mrows"])
        s.op("dve", lambda: nc.vector.tensor_scalar(msel[:, 0, :], mrows[:, 0, :], onehot[0:96, 0:1], None, ALU.mult),
             reads=["mrows", "onehot"], writes=["msel"])
        for q in range(1, 4):
            s.op("dve", lambda q=q: nc.vector.scalar_tensor_tensor(msel[:, 0, :], mrows[:, q, :], onehot[0:96, q:q + 1],
                                                                   msel[:, 0, :], ALU.mult, ALU.add),
                 reads=["mrows", "onehot", "msel"], writes=["msel"])
        s.op("dve", lambda: nc.vector.tensor_copy(msel[:, 1, :], mrows[:, 4, :]), reads=["mrows", "msel"], writes=["msel"])
        for v in range(2):
            ps, pk = bd.next_ps()
            s.transpose(ps[:, 0:96], msel[:, v, :], ident_f[0:96, 0:96], reads=["msel", "ident_f"], writes=[pk])
            s.op("dve", lambda ps=ps, v=v: nc.vector.tensor_copy(modpp[:, v, :], ps[:, 0:96]), reads=[pk], writes=["modpp"])
            s.dma("sp", modsel[v].rearrange("(r p) -> r p", p=128), msel[:, v, :], reads=["msel"], writes=["modsel"])
        s.barrier()

    def norm_T(st_key, src_ap, src_key, Dn, out_sb, out_key, tcol, A_ap, B_ap, tmp, fp32_out=None):
        sq, ss, xh = tmp
        nch = Dn // 128
        s.op("act", lambda: nc.scalar.activation(sq[:, 0:Dn], src_ap, AF.Square), reads=[src_key], writes=["n_sq"])
        s.op("dve", lambda: nc.vector.reduce_sum(ss[:, 0:1], sq[:, 0:Dn], axis=AX.X), reads=["n_sq"], writes=["n_ss"])
        s.op("dve", lambda: nc.vector.tensor_scalar(ss[:, 1:2], ss[:, 0:1], 1.0 / Dn, EPS, ALU.mult, ALU.add),
             reads=["n_ss"], writes=["n_ss"])
        s.op("act", lambda: nc.scalar.activation(ss[:, 2:3], ss[:, 1:2], AF.Sqrt), reads=["n_ss"], writes=["n_ss"])
        s.op("dve", lambda: nc.vector.reciprocal(ss[:, 3:4], ss[:, 2:3]), reads=["n_ss"], writes=["n_ss"])
        if fp32_out is None:
            s.op("dve", lambda: nc.vector.tensor_scalar(xh[:, 0:Dn], src_ap, ss[:, 3:4], None, ALU.mult),
                 reads=[src_key, "n_ss"], writes=["n_xh"])
            for c0 in range(0, nch, 8):
                cn = min(8, nch - c0)
                for c in range(c0, c0 + cn):
                    s.transpose(psT[:, (c - c0) * 128:(c - c0 + 1) * 128], xh[:, c * 128:(c + 1) * 128], ident_b[:],
                                reads=["n_xh", "ident_b"], writes=["psT"])
                for c in range(c0, c0 + cn):
                    pin = psT[:, (c - c0) * 128:(c - c0 + 1) * 128]
                    dst = out_sb[:, c, tcol:tcol + 128]
                    if B_ap is not None:
                        s.op("dve", lambda pin=pin, dst=dst, c=c: nc.vector.tensor_scalar(
                            dst, pin, A_ap[:, c:c + 1], B_ap[:, c:c + 1], ALU.mult, ALU.add),
                            reads=["psT", "ABpp"], writes=[out_key])
                    else:
                        s.op("dve", lambda pin=pin, dst=dst, c=c: nc.vector.tensor_scalar(
                            dst, pin, A_ap[:, c:c + 1], None, ALU.mult), reads=["psT", "ABpp"], writes=[out_key])
        else:
            xf, of32 = fp32_out
            s.op("dve", lambda: nc.vector.tensor_scalar(xf[:, 0:Dn], src_ap, ss[:, 3:4], None, ALU.mult),
                 reads=[src_key, "n_ss"], writes=["n_xf"])
            for c0 in range(0, nch, 4):
                ps, pk = bd.next_ps()
                for c in range(c0, c0 + 4):
                    s.transpose(ps[:, (c - c0) * 128:(c - c0 + 1) * 128], xf[:, c * 128:(c + 1) * 128], ident_f[:],
                                reads=["n_xf", "ident_f"], writes=[pk])
                for c in range(c0, c0 + 4):
                    pin = ps[:, (c - c0) * 128:(c - c0 + 1) * 128]
                    s.op("dve", lambda pin=pin, c=c: nc.vector.tensor_scalar(
                        of32[:, c, :], pin, A_ap[:, c:c + 1], B_ap[:, c:c + 1], ALU.mult, ALU.add),
                        reads=[pk, "ABpp"], writes=["of32"])
                    s.op("act", lambda c=c: nc.scalar.copy(out_sb[:, c, tcol:tcol + 128], of32[:, c, :]),
                         reads=["of32"], writes=[out_key])

    def make_AB(st, gain_e, m_sh, m_sc):
        gpp = bd.sb(st, "gpp", (128, KC), F32)
        AB = bd.sb(st, "AB", (128, 2, 2, KC), F32)
        s.dma("sp", gpp[:], gain_e[l], reads=[], writes=["gpp"])
        for v in range(2):
            s.op("dve", lambda v=v: nc.vector.scalar_tensor_tensor(
                AB[:, v, 0, :], modpp[:, v, m_sc * KC:(m_sc + 1) * KC], 1.0, gpp[:], ALU.add, ALU.mult),
                reads=["modpp", "gpp"], writes=["ABpp"])
            s.op("dve", lambda v=v: nc.vector.tensor_copy(AB[:, v, 1, :], modpp[:, v, m_sh * KC:(m_sh + 1) * KC]),
                 reads=["modpp", "ABpp"], writes=["ABpp"])
        return AB

    def gemm_fm(W, cols, kcn, X, xkey, blocks, wring, wtag, wkey, epi):
        groups = []
        for ci, (c0, n) in enumerate(cols):
            if groups and groups[-1][0] + groups[-1][1] == c0 and groups[-1][1] + n <= 512:
                groups[-1][1] += n
                groups[-1][2].append((ci, c0, n))
            else:
                groups.append([c0, n, [(ci, c0, n)]])

        def load(gi):
            c0, n, _ = groups[gi]
            slot = bd.wslot[wtag] % len(wring)
            bd.wslot[wtag] += 1
            bd.load_w(wring[slot], W, c0, n, kcn, (wtag, slot), wkey)
            return slot
        pre = len(wring) > 1
        slots = {0: load(0)}
        for gi in range(len(groups)):
            if pre and gi + 1 < len(groups):
                slots[gi + 1] = load(gi + 1)
            if not pre and gi > 0:
                slots[gi] = load(gi)
            c0g, ng, members = groups[gi]
            wt = wring[slots[gi]]
            for (ci, c0, n) in members:
                off = c0 - c0g
                for (t0, tn) in blocks:
                    ps, pk = bd.next_ps()
                    pairs = [(wt[:, kc, off:off + n], X[:, kc, t0:t0 + tn]) for kc in range(kcn)]
                    s.mm(ps[0:n, 0:tn], pairs, reads=[(wtag, slots[gi]), xkey], writes=[pk])
                    epi(ci, (t0, tn), ps, pk)

    def gemm_tm(W, c0, ncols, kcn, X, xkey, tiles, wring, wtag, wkey, epi):
        groups = [(c, min(512, c0 + ncols - c)) for c in range(c0, c0 + ncols, 512)]

        def load(gi):
            cg0, n = groups[gi]
            slot = bd.wslot[wtag] % len(wring)
            bd.wslot[wtag] += 1
            bd.load_w(wring[slot], W, cg0, n, kcn, (wtag, slot), wkey)
            return slot
        pre = len(wring) > 1
        slots = {0: load(0)}
        for gi in range(len(groups)):
            if pre and gi + 1 < len(groups):
                slots[gi + 1] = load(gi + 1)
            if not pre and gi > 0:
                slots[gi] = load(gi)
            cg0, n = groups[gi]
            wt = wring[slots[gi]]
            for t in tiles:
                ps, pk = bd.next_ps()
                pairs = [(X[:, kc, t * 128:(t + 1) * 128], wt[:, kc, 0:n]) for kc in range(kcn)]
                s.mm(ps[:, 0:n], pairs, reads=[(wtag, slots[gi]), xkey], writes=[pk])
                epi(gi, t, ps, pk, cg0, n)

    bd.wslot = {"w16": 0, "w4": 0, "wm": 0, "wgu": 0, "wdn": 0, "wf": 0}
    W_in = Wg[("w_in", l)]

    with ExitStack() as st:
        hT = bd.sb(st, "hT", (128, KC, TS), BF16)
        w16 = [bd.sb(st, f"w16_{i}", (128, KC, 512), BF16) for i in range(2)]
        AB = make_AB(st, g["norm1_e"], 0, 1)
        with ExitStack() as st2:
            xt = [bd.sb(st2, f"xt{i}", (128, D), F32) for i in range(2)]
            sq = bd.sb(st2, "sq", (128, D), F32)
            ss = bd.sb(st2, "ss", (128, 4), F32)
            xh = bd.sb(st2, "xh", (128, D), BF16)
            for t in range(NT_ALL):
                xb = xt[t % 2]
                s.dma("sp", xb[:], xcur[t * 128:(t + 1) * 128, :], reads=["xcur"], writes=[("xt", t % 2)])
                v = 1 if t < 2 else 0
                norm_T(st2, xb[:], ("xt", t % 2), D, hT, "hT", t * 128, AB[:, v, 0, :], AB[:, v, 1, :], (sq, ss, xh))
            s.barrier()
        s.dma("sp", fm_view(hfm), hT[:, :, 0:TOWN], reads=["hT"], writes=["hfm"])

        with ExitStack() as st2:
            sq = bd.sb(st2, "sq", (128, 512), F32)
            ss = bd.sb(st2, "ss", (128, 4), F32)
            xh = bd.sb(st2, "xh", (128, 512), BF16)
            qg = bd.sb(st2, "qg", (128, 2, 4), F32)
            s.dma("sp", qg[:, 0, :], g["qn_e"][l], writes=["ABpp"])
            s.dma("sp", qg[:, 1, :], g["kvn_e"][l], reads=["ABpp"], writes=["ABpp"])
            stg = bd.sb(st2, "stg", (128, 4, TS), BF16)
            gemm_tm(W_in, O_CQ, 512, KC, hT, "hT", range(T0 // 128, NT_OWN), w16, "w16", "w_in_%d_all" % l,
                    lambda gi, t, ps, pk, c0, n: norm_T(st2, ps[:, 0:512], pk, 512, stg, "stg", t * 128, qg[:, 0, :], None, (sq, ss, xh)))
            s.dma("sp", fm_view(cqn_d), stg[:, :, 0:TOWN], reads=["stg"], writes=["cqn_d"])
            gemm_tm(W_in, O_CKV, 512, KC, hT, "hT", range(NT_ALL), w16, "w16", "w_in_%d_all" % l,
                    lambda gi, t, ps, pk, c0, n: norm_T(st2, ps[:, 0:512], pk, 512, stg, "stg", t * 128, qg[:, 1, :], None, (sq, ss, xh)))
            s.dma("sp", fm_view(ckvn_d), stg[:, :, :], reads=["stg"], writes=["ckvn_d"])
            s.barrier()

        with ExitStack() as st2:
            wkr = bd.sb(st2, "wkr", (128, KC, 128), BF16)
            rC = bd.sb(st2, "rC", (64, TS), F32)
            rS = bd.sb(st2, "rS", (64, TS), F32)
            krs = bd.sb(st2, "krs", (64, TS), BF16)
            tmp = [bd.sb(st2, f"krt{i}", (64, 512), F32) for i in range(2)]
            s.dma("sp", rC[:], g["ropeC_e"], writes=["rC"])
            s.dma("sp", rS[:], g["ropeS_e"], writes=["rS"])
            wk = "w_in_%d_all" % l
            for (dst0, src0, n) in ((0, O_KR, 64), (64, O_KR + 32, 32), (96, O_KR, 32)):
                bd.wload(wkr[:, :, dst0:dst0 + n], W_in[:, src0:src0 + n], "wkr", wk)
            for (t0, tn) in bd.tok_blocks(0, TS):
                p1, k1 = bd.next_ps()
                p2, k2 = bd.next_ps()
                s.mm(p1[0:64, 0:tn], [(wkr[:, kc, 0:64], hT[:, kc, t0:t0 + tn]) for kc in range(KC)], reads=["wkr", "hT"], writes=[k1])
                s.mm(p2[0:64, 0:tn], [(wkr[:, kc, 64:128], hT[:, kc, t0:t0 + tn]) for kc in range(KC)], reads=["wkr", "hT"], writes=[k2])
                s.op("dve", lambda p1=p1, t0=t0, tn=tn: nc.vector.tensor_tensor(tmp[0][:, 0:tn], p1[0:64, 0:tn], rC[:, t0:t0 + tn], ALU.mult),
                     reads=[k1, "rC"], writes=["krt0"])
                s.op("dve", lambda p2=p2, t0=t0, tn=tn: nc.vector.tensor_tensor(tmp[1][:, 0:tn], p2[0:64, 0:tn], rS[:, t0:t0 + tn], ALU.mult),
                     reads=[k2, "rS"], writes=["krt1"])
                s.op("dve", lambda t0=t0, tn=tn: nc.vector.tensor_tensor(krs[:, t0:t0 + tn], tmp[0][:, 0:tn], tmp[1][:, 0:tn], ALU.add),
                     reads=["krt0", "krt1"], writes=["krs"])
            s.dma("sp", kr_d, krs[:], reads=["krs"], writes=["kr_d"])
            s.barrier()

        with ExitStack() as st2:
            fst = [bd.sb(st2, f"fst{i}", (128, 512), BF16) for i in range(2)]
            cnt = [0]

            def epi_f(gi, t, ps, pk, c0, n):
                b_ = fst[cnt[0] % 2]
                k_ = ("fst", cnt[0] % 2)
                cnt[0] += 1
                s.op("act", lambda: nc.scalar.copy(b_[:, 0:n], ps[:, 0:n]), reads=[pk], writes=[k_])
                s.dma("sp", fz_d[t * 128:(t + 1) * 128, c0 - O_F:c0 - O_F + n], b_[:, 0:n], reads=[k_], writes=["fz_d"])
            gemm_tm(W_in, O_F, 1024, KC, hT, "hT", range(NT_ALL), w16, "w16", "w_in_%d_all" % l, epi_f)
            s.barrier()

        with ExitStack() as st2:
            acc = bd.sb(st2, "acc", (128, 8, TOWN), F32)
            zb = [bd.sb(st2, "zb0", (128, TOWN + 60), F32)] * 2
            sg = [bd.sb(st2, f"sg{i}", (128, 512), F32) for i in range(2)]
            cw = bd.sb(st2, "cw", (128, 8, 31), F32)
            cb = bd.sb(st2, "cb", (128, 3, 8), F32)
            s.dma("sp", cw[:].rearrange("p c k -> p (c k)"), g["cdw_e"][l], writes=["cw"])
            s.dma("sp", cb[:, 0, :], g["cdb_e"][l], writes=["cb"])
            s.dma("sp", cb[:, 1, :], g["clg_e"][l], reads=["cb"], writes=["cb"])
            s.dma("sp", cb[:, 2, :], g["clb_e"][l], reads=["cb"], writes=["cb"])
            ZC0, ZL0 = 15, 15 + 256 + 15 + 15
            ublocks = [(0, 256, ZC0, None), (256, 512, ZL0, None), (768, 512, ZL0 + 512, None),
                       (TS - 15, 15, ZL0 - 15, 0), (TOWN, 15, ZL0 + 1024, 1)]
            if last:
                ublocks = ublocks[1:]
            sgc = [0]
            for c in range(8):
                z = zb[c % 2]
                zk = ("zb", 0)
                s.op("dve", lambda z=z: nc.vector.memset(z[:], 0.0), reads=[], writes=[zk])
                slot = bd.wslot["w16"] % 2
                bd.wslot["w16"] += 1
                wt = w16[slot]
                bd.load_w(wt[:, :, 0:128], W_in, O_U + c * 128, 128, KC, ("w16", slot), wk)
                bd.wload(wt[:, :, 128:256], W_in[:, O_U + 1024 + c * 128:O_U + 1024 + (c + 1) * 128], ("w16", slot), wk)
                for (t0, tn, z0, hm) in ublocks:
                    pa, ka = bd.next_ps()
                    pg, kg = bd.next_ps()
                    s.mm(pa[:, 0:tn], [(wt[:, kc, 0:128], hT[:, kc, t0:t0 + tn]) for kc in range(KC)], reads=[("w16", slot), "hT"], writes=[ka])
                    s.mm(pg[:, 0:tn], [(wt[:, kc, 128:256], hT[:, kc, t0:t0 + tn]) for kc in range(KC)], reads=[("w16", slot), "hT"], writes=[kg])
                    sb_ = sg[sgc[0] % 2]
                    sk = ("sg", sgc[0] % 2)
                    sgc[0] += 1
                    s.op("act", lambda pg=pg, sb_=sb_, tn=tn: nc.scalar.activation(sb_[:, 0:tn], pg[:, 0:tn], AF.Sigmoid), reads=[kg], writes=[sk])
                    s.op("dve", lambda pa=pa, sb_=sb_, z=z, z0=z0, tn=tn: nc.vector.tensor_tensor(z[:, z0:z0 + tn], pa[:, 0:tn], sb_[:, 0:tn], ALU.mult),
                         reads=[ka, sk], writes=[zk])
                    if hm is not None:
                        s.op("dve", lambda z=z, z0=z0, tn=tn, hm=hm: nc.vector.tensor_scalar(z[:, z0:z0 + tn], z[:, z0:z0 + tn], halo_m[:, hm:hm + 1], None, ALU.mult),
                             reads=[zk, "halo_m"], writes=[zk])
                for (a0, an, zs) in (((0, 256, ZC0 - 15), (256, 1024, ZL0 - 15))[(1 if last else 0):]):
                    s.op("dve", lambda z=z, c=c, a0=a0, an=an, zs=zs: nc.vector.tensor_scalar(
                        acc[:, c, a0:a0 + an], z[:, zs:zs + an], cw[:, c, 0:1], cb[:, 0, c:c + 1], ALU.mult, ALU.add),
                        reads=[zk, "cw", "cb"], writes=[("acc", c)])
                    for k in range(1, 31):
                        s.op("dve", lambda z=z, c=c, a0=a0, an=an, zs=zs, k=k: nc.vector.scalar_tensor_tensor(
                            acc[:, c, a0:a0 + an], z[:, zs + k:zs + k + an], cw[:, c, k:k + 1], acc[:, c, a0:a0 + an], ALU.mult, ALU.add),
                            reads=[zk, "cw", ("acc", c)], writes=[("acc", c)])
            mean = bd.sb(st2, "mean", (128, 512), F32)
            rstd = bd.sb(st2, "rstd", (128, 512), F32)
            sqc = [bd.sb(st2, f"sqc{i}", (128, 512), F32) for i in range(2)]
            cst = [bd.sb(st2, f"cst{i}", (128, 512), BF16) for i in range(2)]
            cstn = [0]
            for (t0, tn) in own_blocks:
                pm, km = bd.next_ps()
                pq, kq = bd.next_ps()
                s.mm(pm[:, 0:tn], [(ones_f[:], acc[:, c, t0:t0 + tn]) for c in range(8)], reads=["ones_f", "acc"], writes=[km])
                for c in range(8):
                    q_ = sqc[c % 2]
                    s.op("act", lambda q_=q_, c=c, t0=t0, tn=tn: nc.scalar.activation(q_[:, 0:tn], acc[:, c, t0:t0 + tn], AF.Square),
                         reads=["acc"], writes=[("sqc", c % 2)])
                    s.mm(pq[:, 0:tn], [(ones_f[:], q_[:, 0:tn])], reads=["ones_f", ("sqc", c % 2)], writes=[kq], start=(c == 0), stop=(c == 7))
                s.op("dve", lambda pm=pm, tn=tn: nc.vector.tensor_scalar(mean[:, 0:tn], pm[:, 0:tn], 1.0 / 1024, None, ALU.mult), reads=[km], writes=["mean"])
                s.op("dve", lambda tn=tn: nc.vector.tensor_tensor(rstd[:, 0:tn], mean[:, 0:tn], mean[:, 0:tn], ALU.mult), reads=["mean"], writes=["rstd"])
                s.op("dve", lambda pq=pq, tn=tn: nc.vector.scalar_tensor_tensor(rstd[:, 0:tn], pq[:, 0:tn], 1.0 / 1024, rstd[:, 0:tn], ALU.mult, ALU.subtract),
                     reads=[kq, "rstd"], writes=["rstd"])
                s.op("dve", lambda tn=tn: nc.vector.tensor_scalar(rstd[:, 0:tn], rstd[:, 0:tn], EPS, None, ALU.add), reads=["rstd"], writes=["rstd"])
                s.op("act", lambda tn=tn: nc.scalar.activation(rstd[:, 0:tn], rstd[:, 0:tn], AF.Sqrt), reads=["rstd"], writes=["rstd"])
                s.op("dve", lambda tn=tn: nc.vector.reciprocal(rstd[:, 0:tn], rstd[:, 0:tn]), reads=["rstd"], writes=["rstd"])
                for c in range(8):
                    s.op("dve", lambda c=c, t0=t0, tn=tn: nc.vector.tensor_tensor(acc[:, c, t0:t0 + tn], acc[:, c, t0:t0 + tn], mean[:, 0:tn], ALU.subtract),
                         reads=["acc", "mean"], writes=["acc"])
                    s.op("dve", lambda c=c, t0=t0, tn=tn: nc.vector.tensor_tensor(acc[:, c, t0:t0 + tn], acc[:, c, t0:t0 + tn], rstd[:, 0:tn], ALU.mult),
                         reads=["acc", "rstd"], writes=["acc"])
                    cb_ = cst[cstn[0] % 2]
                    ck_ = ("cst", cstn[0] % 2)
                    cstn[0] += 1
                    s.op("act", lambda c=c, t0=t0, tn=tn, cb_=cb_: nc.scalar.activation(cb_[:, 0:tn], acc[:, c, t0:t0 + tn], AF.Silu,
                                                                                     bias=cb[:, 2, c:c + 1], scale=cb[:, 1, c:c + 1]),
                         reads=["acc", "cb"], writes=[ck_])
                    s.dma("sp", convfm[c][:, t0:t0 + tn], cb_[:, 0:tn], reads=[ck_], writes=["convfm"])
            s.barrier()
    s.barrier()
    if bd.stop_after == ("mix1", l):
        return
    layer_part2(bd, l, last, env, locals())


def layer_part2(bd, l, last, env, loc):
    nc, s = bd.nc, bd.s
    g = env
    Wg = g["Wg"]
    xcur, xmid, hfm, mfm, h2fm = g["xcur"], g["xmid"], g["hfm"], g["mfm"], g["h2fm"]
    ident_b, ident_f, ones_b, ones_f = g["ident_b"], g["ident_f"], g["ones_b"], g["ones_f"]
    modpp, psT = g["modpp"], g["psT"]
    cqn_d, ckvn_d, kr_d, fz_d, convfm, fourfm, afm = (loc[k] for k in ("cqn_d", "ckvn_d", "kr_d", "fz_d", "convfm", "fourfm", "afm"))
    gemm_fm, gemm_tm, norm_T, make_AB, own_blocks = loc["gemm_fm"], loc["gemm_tm"], loc["norm_T"], loc["make_AB"], loc["own_blocks"]
    NT_OWN, NT_ALL = TOWN // 128, TS // 128

    with ExitStack() as st:
        fz = bd.sb(st, "fz", (128, NT_ALL, 1024), BF16)
        s.dma("sp", fz[:], fz_d.rearrange("(t p) c -> p t c", p=128), reads=["fz_d"], writes=["fz"])
        pcs = bd.sb(st, "pcs", (128, 2, 8, TOWN), BF16)
        cring = [bd.sb(st, f"cpos{i}", (128, 16, 512), BF16) for i in range(2)]
        chm = bd.sb(st, "chm", (128, 2, 2, 256), BF16)
        s.dma("sp", chm[:, 0, :, :], g["chC_e"].rearrange("(k p) m -> p k m", p=128), writes=["chm"])
        s.dma("sp", chm[:, 1, :, :], g["chSn_e"].rearrange("(k p) m -> p k m", p=128), reads=["chm"], writes=["chm"])
        n = 0
        for (cs, (Ce, Se), ntile0, nk, kblocks, kout0) in ((
                ("ctx", (g["cposC_e"], g["cposS_e"]), 0, 2, [(0, 256)], 0),
                ("lat", (g["posC_e"], g["posS_e"]), 2, 16, [(0, 512), (512, 512)], 256))[(1 if last else 0):]):
            for ti, tab in enumerate((Ce, Se)):
                for (k0, kn) in kblocks:
                    ct = cring[n % 2]
                    ck = ("cpos", n % 2)
                    n += 1
                    s.dma("sp", ct[:, 0:nk, 0:kn], tab[:, k0:k0 + kn].rearrange("(k p) m -> p k m", p=128), reads=[ck], writes=[ck])
                    for c in range(8):
                        ps, pk = bd.next_ps()
                        s.mm(ps[:, 0:kn], [(fz[:, ntile0 + j, c * 128:(c + 1) * 128], ct[:, j, 0:kn]) for j in range(nk)],
                             reads=["fz", ck], writes=[pk])
                        s.op("act", lambda ps=ps, ti=ti, c=c, k0=k0, kn=kn, kout0=kout0: nc.scalar.copy(
                            pcs[:, ti, c, kout0 + k0:kout0 + k0 + kn], ps[:, 0:kn]), reads=[pk], writes=["pcs"])
        fst = bd.sb(st, "fst", (128, 8, TOWN), BF16)
        for grp in range(4):
            for mh in range(2):
                for (t0, tn) in own_blocks:
                    ps, pk = bd.next_ps()
                    pairs = []
                    for ti in range(2):
                        for ch in range(2):
                            pairs.append((chm[:, ti, ch, mh * 128:(mh + 1) * 128], pcs[:, ti, grp * 2 + ch, t0:t0 + tn]))
                    s.mm(ps[:, 0:tn], pairs, reads=["chm", "pcs"], writes=[pk])
                    s.op("act", lambda ps=ps, grp=grp, mh=mh, t0=t0, tn=tn: nc.scalar.copy(fst[:, grp * 2 + mh, t0:t0 + tn], ps[:, 0:tn]),
                         reads=[pk], writes=["fst"])
        s.dma("sp", fm_view(fourfm), fst[:], reads=["fst"], writes=["fourfm"])
        s.barrier()

    W_uq, W_ukv = Wg[("w_uq", l)], Wg[("w_ukv", l)]
    with ExitStack() as st:
        cqn = bd.sb(st, "cqn", (128, 4, TOWN), BF16)
        ckvn = bd.sb(st, "ckvn", (128, 4, TS), BF16)
        krT = bd.sb(st, "krT", (64, TS), BF16)
        rC = bd.sb(st, "rCq", (64, TOWN), F32)
        rS = bd.sb(st, "rSq", (64, TOWN), F32)
        s.dma("sp", cqn[:], fm_view(cqn_d), reads=["cqn_d"], writes=["cqn"])
        s.dma("sp", ckvn[:], fm_view(ckvn_d), reads=["ckvn_d"], writes=["ckvn"])
        s.dma("sp", krT[:], kr_d, reads=["kr_d"], writes=["krT"])
        s.dma("sp", rC[:], g["ropeC_e"][:, 0:TOWN], writes=["rCq"])
        s.dma("sp", rS[:], g["ropeS_e"][:, 0:TOWN], writes=["rSq"])
        wq = [bd.sb(st, f"wq{i}", (128, 4, 256), BF16) for i in range(2)]
        wkv = [bd.sb(st, f"wkv{i}", (128, 4, 256), BF16) for i in range(2)]
        qT = bd.sb(st, "qT", (128, TOWN), BF16)
        qrT = bd.sb(st, "qrT", (64, TOWN), BF16)
        kT = bd.sb(st, "kT", (128, TS), BF16)
        V = bd.sb(st, "V", (128, NT_ALL, 128), BF16)
        pT = [bd.sb(st, f"pT{i}", (128, 512), BF16) for i in range(3)]
        tq = [bd.sb(st, f"tq{i}", (64, 512), F32) for i in range(2)]
        rec = bd.sb(st, "rec", (128, 512), F32)
        ast = [bd.sb(st, f"ast{i}", (128, TOWN), BF16) for i in range(2)]
        wuk, wkk = "w_uq_%d_all" % l, "w_ukv_%d_all" % l
        pn = 0
        for h in range(HEADS):
            wqt, wkvt = wq[h % 2], wkv[h % 2]
            qk_, kvk_ = ("wq", h % 2), ("wkv", h % 2)
            b0 = h * 192
            for (d0, s0, n) in ((0, b0, 192), (192, b0 + 160, 32), (224, b0 + 128, 32)):
                bd.wload(wqt[:, :, d0:d0 + n], W_uq[:, s0:s0 + n], qk_, wuk)
            bd.wload(wkvt[:], W_ukv[:, h * 256:(h + 1) * 256], kvk_, wkk)
            for (t0, tn) in own_blocks:
                ps, pk = bd.next_ps()
                s.mm(ps[:, 0:tn], [(wqt[:, kc, 0:128], cqn[:, kc, t0:t0 + tn]) for kc in range(4)], reads=[qk_, "cqn"], writes=[pk])
                s.op("act", lambda ps=ps, t0=t0, tn=tn: nc.scalar.copy(qT[:, t0:t0 + tn], ps[:, 0:tn]), reads=[pk], writes=["qT"])
                p1, k1 = bd.next_ps()
                p2, k2 = bd.next_ps()
                s.mm(p1[0:64, 0:tn], [(wqt[:, kc, 128:192], cqn[:, kc, t0:t0 + tn]) for kc in range(4)], reads=[qk_, "cqn"], writes=[k1])
                s.mm(p2[0:64, 0:tn], [(wqt[:, kc, 192:256], cqn[:, kc, t0:t0 + tn]) for kc in range(4)], reads=[qk_, "cqn"], writes=[k2])
                s.op("dve", lambda p1=p1, t0=t0, tn=tn: nc.vector.tensor_tensor(tq[0][:, 0:tn], p1[0:64, 0:tn], rC[:, t0:t0 + tn], ALU.mult), reads=[k1, "rCq"], writes=["tq0"])
                s.op("dve", lambda p2=p2, t0=t0, tn=tn: nc.vector.tensor_tensor(tq[1][:, 0:tn], p2[0:64, 0:tn], rS[:, t0:t0 + tn], ALU.mult), reads=[k2, "rSq"], writes=["tq1"])
                s.op("dve", lambda t0=t0, tn=tn: nc.vector.tensor_tensor(qrT[:, t0:t0 + tn], tq[0][:, 0:tn], tq[1][:, 0:tn], ALU.add), reads=["tq0", "tq1"], writes=["qrT"])
            for (t0, tn) in bd.tok_blocks(0, TS):
                ps, pk = bd.next_ps()
                s.mm(ps[:, 0:tn], [(wkvt[:, kc, 0:128], ckvn[:, kc, t0:t0 + tn]) for kc in range(4)], reads=[kvk_, "ckvn"], writes=[pk])
                s.op("act", lambda ps=ps, t0=t0, tn=tn: nc.scalar.copy(kT[:, t0:t0 + tn], ps[:, 0:tn]), reads=[pk], writes=["kT"])
            for t4 in range(0, NT_ALL, 4):
                ps, pk = bd.next_ps()
                nt = min(4, NT_ALL - t4)
                for j in range(nt):
                    t = t4 + j
                    s.mm(ps[:, j * 128:(j + 1) * 128], [(ckvn[:, kc, t * 128:(t + 1) * 128], wkvt[:, kc, 128:256]) for kc in range(4)],
                         reads=[kvk_, "ckvn"], writes=[pk])
                s.op("dve", lambda ps=ps, t4=t4, nt=nt: nc.vector.tensor_copy(V[:, t4:t4 + nt, :].rearrange("p t d -> p (t d)"), ps[:, 0:nt * 128]),
                     reads=[pk], writes=["V"])
            ab = ast[h % 2]
            ak = ("ast", h % 2)
            for qi, (q0, qn, ktiles) in enumerate(((0, 256, range(0, 2)), (256, 512, range(0, NT_ALL)), (768, 512, range(0, NT_ALL)))[(1 if last else 0):]):
                bd.ps_n = 3
                bsel = 3 + 2 * ((h * 3 + qi) % 2)
                po, ko = bd.ps[bsel], ("ps", bsel)
                pz, kz = bd.ps[bsel + 1], ("ps", bsel + 1)
                kl = list(ktiles)
                for i, kt in enumerate(kl):
                    psc, ksc = bd.next_ps()
                    s.mm(psc[:, 0:qn], [(kT[:, kt * 128:(kt + 1) * 128], qT[:, q0:q0 + qn]), (krT[:, kt * 128:(kt + 1) * 128], qrT[:, q0:q0 + qn])],
                         reads=["kT", "qT", "krT", "qrT"], writes=[ksc])
                    pb = pT[pn % 3]
                    pkk = ("pT", pn % 3)
                    pn += 1
                    s.op("act", lambda psc=psc, pb=pb, qn=qn: nc.scalar.activation(pb[:, 0:qn], psc[:, 0:qn], AF.Exp, scale=ATT_SCALE),
                         reads=[ksc], writes=[pkk])
                    s.mm(po[:, 0:qn], [(V[:, kt, :], pb[:, 0:qn])], reads=["V", pkk], writes=[ko], start=(i == 0), stop=(i == len(kl) - 1))
                    s.mm(pz[:, 0:qn], [(ones_b[:], pb[:, 0:qn])], reads=["ones_b", pkk], writes=[kz], start=(i == 0), stop=(i == len(kl) - 1))
                s.op("dve", lambda pz=pz, qn=qn: nc.vector.reciprocal(rec[:, 0:qn], pz[:, 0:qn]), reads=[kz], writes=["rec"])
                s.op("dve", lambda po=po, q0=q0, qn=qn, ab=ab: nc.vector.tensor_tensor(ab[:, q0:q0 + qn], po[:, 0:qn], rec[:, 0:qn], ALU.mult),
                     reads=[ko, "rec"], writes=[ak])
            bd.ps_n = 7
            s.dma("sp", afm[h], ab[:], reads=[ak], writes=["afm"])
        s.barrier()
    if bd.stop_after == ("att", l):
        return
    layer_part3(bd, l, last, env, loc)


def layer_part3(bd, l, last, env, loc):
    nc, s = bd.nc, bd.s
    g = env
    Wg = g["Wg"]
    xcur, xmid, hfm, mfm, h2fm = g["xcur"], g["xmid"], g["hfm"], g["mfm"], g["h2fm"]
    ident_b, ident_f, ones_b, ones_f = g["ident_b"], g["ident_f"], g["ones_b"], g["ones_f"]
    modpp, psT = g["modpp"], g["psT"]
    convfm, fourfm, afm = loc["convfm"], loc["fourfm"], loc["afm"]
    gemm_fm, gemm_tm, norm_T, make_AB, own_blocks = loc["gemm_fm"], loc["gemm_tm"], loc["norm_T"], loc["make_AB"], loc["own_blocks"]
    NT_OWN = TOWN // 128
    W_in = Wg[("w_in", l)]

    with ExitStack() as st:
        hT = bd.sb(st, "hTo", (128, KC, TOWN), BF16)
        aT = bd.sb(st, "aT", (128, KC, TOWN), BF16)
        cT_ = bd.sb(st, "cvT", (128, 8, TOWN), BF16)
        fT = bd.sb(st, "fT", (128, 8, TOWN), BF16)
        s.dma("sp", hT[:], fm_view(hfm), reads=["hfm"], writes=["hTo"])
        s.dma("sp", aT[:], fm_view(afm), reads=["afm"], writes=["aT"])
        s.dma("sp", cT_[:], fm_view(convfm), reads=["convfm"], writes=["cvT"])
        s.dma("sp", fT[:], fm_view(fourfm), reads=["fourfm"], writes=["fT"])
        wm = [bd.sb(st, f"wm{i}", (128, 80, 128), BF16) for i in range(2)]
        sgm = [bd.sb(st, f"sgm{i}", (128, 512), F32) for i in range(3)]
        mt = [bd.sb(st, f"mt{i}", (128, 512), F32) for i in range(2)]
        mst = [bd.sb(st, "mst0", (128, TOWN), BF16)] * 2
        srcs = [(Wg[("w_mla_out", l)], 0, 16, "w_mla_out_%d_all" % l), (Wg[("w_conv_out", l)], 0, 8, "w_conv_out_%d_all" % l),
                (Wg[("w_four_out", l)], 0, 8, "w_four_out_%d_all" % l),
                (W_in, O_G, 16, "w_in_%d_all" % l), (W_in, O_G + D, 16, "w_in_%d_all" % l), (W_in, O_G + 2 * D, 16, "w_in_%d_all" % l)]
        koff = [0, 16, 24, 32, 48, 64]
        xin = [(aT, "aT"), (cT_, "cvT"), (fT, "fT"), (hT, "hTo"), (hT, "hTo"), (hT, "hTo")]

        def loadj(j):
            wt = wm[j % 2]
            for i, (W, cbase, kcn, wkey) in enumerate(srcs):
                bd.wload(wt[:, koff[i]:koff[i] + kcn, :], W[:, cbase + j * 128:cbase + (j + 1) * 128], ("wm", j % 2), wkey)
        loadj(0)
        for j in range(KC):
            if j + 1 < KC:
                loadj(j + 1)
            wt = wm[j % 2]
            wk = ("wm", j % 2)
            mb = mst[j % 2]
            mk = ("mst", 0)
            for (t0, tn) in own_blocks:
                pss = []
                for i in range(6):
                    ps, pk = bd.next_ps()
                    X, xk = xin[i]
                    kcn = srcs[i][2]
                    s.mm(ps[:, 0:tn], [(wt[:, koff[i] + kc, :], X[:, kc, t0:t0 + tn]) for kc in range(kcn)], reads=[wk, xk], writes=[pk])
                    pss.append((ps, pk))
                for i in range(3):
                    s.op("act", lambda i=i, tn=tn: nc.scalar.activation(sgm[i][:, 0:tn], pss[3 + i][0][:, 0:tn], AF.Sigmoid),
                         reads=[pss[3 + i][1]], writes=[("sgm", i)])
                s.op("dve", lambda tn=tn: nc.vector.tensor_tensor(mt[0][:, 0:tn], pss[0][0][:, 0:tn], sgm[0][:, 0:tn], ALU.mult),
                     reads=[pss[0][1], ("sgm", 0)], writes=["mt0"])
                for i in (1, 2):
                    s.op("dve", lambda i=i, tn=tn: nc.vector.tensor_tensor(mt[1][:, 0:tn], pss[i][0][:, 0:tn], sgm[i][:, 0:tn], ALU.mult),
                         reads=[pss[i][1], ("sgm", i)], writes=["mt1"])
                    if i == 1:
                        s.op("dve", lambda tn=tn: nc.vector.tensor_tensor(mt[0][:, 0:tn], mt[0][:, 0:tn], mt[1][:, 0:tn], ALU.add),
                             reads=["mt0", "mt1"], writes=["mt0"])
                    else:
                        s.op("dve", lambda tn=tn, t0=t0, mb=mb: nc.vector.tensor_tensor(mb[:, t0:t0 + tn], mt[0][:, 0:tn], mt[1][:, 0:tn], ALU.add),
                             reads=["mt0", "mt1"], writes=[mk])
            s.dma("sp", mfm[j], mb[:], reads=[mk], writes=["mfm"])
        s.barrier()

    with ExitStack() as st:
        mT = bd.sb(st, "mT", (128, KC, TOWN), BF16)
        s.dma("sp", mT[:], fm_view(mfm), reads=["mfm"], writes=["mT"])
        w16 = [bd.sb(st, f"w16_{i}", (128, KC, 512), BF16) for i in range(2)]
        xo = bd.sb(st, "xo", (128, NT_OWN, D), F32)
        s.dma("sp", xo[:], xcur[0:TOWN, :].rearrange("(t p) d -> p t d", p=128), reads=["xcur"], writes=["xo"])
        tmp = [bd.sb(st, f"rt{i}", (128, 512), F32) for i in range(2)]
        cn = [0]
        g_bc = bd.sb(st, "g_bc", (128, 2, D), F32)
        for v in range(2):
            s.dma("sp", g_bc[:, v, :], bcast_rows(loc["modsel"][v:v + 1, 2 * 2048:3 * 2048]), reads=["modsel"], writes=["g_bc"])

        def epi_o(gi, t, ps, pk, c0, n):
            tb = tmp[cn[0] % 2]
            tk = ("rt", cn[0] % 2)
            cn[0] += 1
            v = 1 if t < 2 else 0
            s.op("dve", lambda: nc.vector.tensor_tensor(tb[:, 0:n], ps[:, 0:n], g_bc[:, v, c0:c0 + n], ALU.mult), reads=[pk, "g_bc"], writes=[tk])
            s.op("dve", lambda: nc.vector.tensor_tensor(xo[:, t, c0:c0 + n], xo[:, t, c0:c0 + n], tb[:, 0:n], ALU.add), reads=[tk, ("xo", t)], writes=[("xo", t)])
        gemm_tm(Wg[("w_out", l)], 0, D, KC, mT, "mT", range((CTX if last else 0) // 128, NT_OWN), w16, "w16", "w_out_%d_all" % l, epi_o)
        s.dma("sp", xmid.rearrange("(t p) d -> p t d", p=128), xo[:], reads=["xo"], writes=["xmid"])
        s.barrier()
    if bd.stop_after == ("mixer", l):
        return

    gates_d = bd.dram(f"gates_d{l}", (TOWN, NE))
    gatesT_d = bd.dram(f"gatesT_d{l}", (NE, TOWN))
    with ExitStack() as st:
        h2 = bd.sb(st, "h2", (128, KC, TOWN), BF16)
        AB = make_AB(st, g["norm2_e"], 3, 4)
        xt = [bd.sb(st, f"xt{i}", (128, D), F32) for i in range(2)]
        sq = bd.sb(st, "sq", (128, D), F32)
        ss = bd.sb(st, "ss", (128, 4), F32)
        xf = bd.sb(st, "xf", (128, D), F32)
        of32 = bd.sb(st, "of32", (128, KC, 128), F32)
        wr = bd.sb(st, "wr", (128, KC, NE), F32)
        brb = bd.sb(st, "brb", (128, NE), F32)
        lg = bd.sb(st, "lg", (128, NE), F32)
        m8 = bd.sb(st, "m8", (128, 8), F32)
        ex = bd.sb(st, "ex", (128, NE), F32)
        msk = bd.sb(st, "msk", (128, NE), F32)
        sm = bd.sb(st, "sm", (128, 2), F32)
        gt = bd.sb(st, "gt", (128, NT_OWN, NE), F32)
        gtT = bd.sb(st, "gtT", (NE, TOWN), F32)
        s.dma("sp", wr[:].rearrange("p k e -> p (k e)"), g["wrt_e"][l], writes=["wr"])
        s.dma("sp", brb[:], g["brt_e"][l], writes=["brb"])
        for t in range((CTX if last else 0) // 128, NT_OWN):
            xb = xt[t % 2]
            s.dma("sp", xb[:], xmid[t * 128:(t + 1) * 128, :], reads=["xmid"], writes=[("xt", t % 2)])
            v = 1 if t < 2 else 0
            norm_T(st, xb[:], ("xt", t % 2), D, h2, "h2", t * 128, AB[:, v, 0, :], AB[:, v, 1, :], (sq, ss, None), fp32_out=(xf, of32))
            ps, pk = bd.next_ps()
            s.mm(ps[:, 0:NE], [(of32[:, kc, :], wr[:, kc, :]) for kc in range(KC)], reads=["of32", "wr"], writes=[pk])
            s.op("dve", lambda ps=ps: nc.vector.tensor_tensor(lg[:], ps[:, 0:NE], brb[:], ALU.add), reads=[pk, "brb"], writes=["lg"])
            s.op("dve", lambda: nc.vector.max(m8[:], lg[:]), reads=["lg"], writes=["m8"])
            s.op("dve", lambda: nc.vector.tensor_scalar(msk[:], lg[:], m8[:, 3:4], None, ALU.is_ge), reads=["lg", "m8"], writes=["msk"])
            s.op("dve", lambda: nc.vector.tensor_scalar(sm[:, 0:1], m8[:, 0:1], -1.0, None, ALU.mult), reads=["m8"], writes=["sm"])
            s.op("act", lambda: nc.scalar.activation(ex[:], lg[:], AF.Exp, bias=sm[:, 0:1], scale=1.0), reads=["lg", "sm"], writes=["ex"])
            s.op("dve", lambda: nc.vector.tensor_tensor(ex[:], ex[:], msk[:], ALU.mult), reads=["ex", "msk"], writes=["ex"])
            s.op("dve", lambda: nc.vector.reduce_sum(sm[:, 1:2], ex[:], axis=AX.X), reads=["ex", "sm"], writes=["sm"])
            s.op("dve", lambda: nc.vector.reciprocal(sm[:, 1:2], sm[:, 1:2]), reads=["sm"], writes=["sm"])
            s.op("dve", lambda t=t: nc.vector.tensor_scalar(gt[:, t, :], ex[:], sm[:, 1:2], None, ALU.mult), reads=["ex", "sm"], writes=["gt"])
            ps2, pk2 = bd.next_ps()
            s.transpose(ps2[0:NE, 0:128], gt[:, t, :], ident_f[:], reads=["gt", "ident_f"], writes=[pk2])
            s.op("dve", lambda ps2=ps2, t=t: nc.vector.tensor_copy(gtT[:, t * 128:(t + 1) * 128], ps2[0:NE, 0:128]), reads=[pk2], writes=["gtT"])
        s.dma("sp", fm_view(h2fm), h2[:], reads=["h2"], writes=["h2fm"])
        s.dma("sp", gates_d.rearrange("(t p) e -> p t e", p=128), gt[:], reads=["gt"], writes=["gates_d"])
        s.dma("sp", gatesT_d, gtT[:], reads=["gtT"], writes=["gatesT_d"])
        s.barrier()

    NPASS = 2
    TM0 = CTX if last else 0
    PT = (TOWN - TM0) // NPASS
    for ps_i in range(NPASS):
        p0 = TM0 + ps_i * PT
        blocks = bd.tok_blocks(p0, p0 + PT)
        ptiles = range(p0 // 128, (p0 + PT) // 128)
        with ExitStack() as st:
            h2 = bd.sb(st, "h2p", (128, KC, PT), BF16)
            s.dma("sp", h2[:], fm_view(h2fm)[:, :, p0:p0 + PT], reads=["h2fm"], writes=["h2p"])
            gt = bd.sb(st, "gtp", (128, PT // 128, NE), F32)
            s.dma("sp", gt[:], gates_d[p0:p0 + PT, :].rearrange("(t p) e -> p t e", p=128), reads=["gates_d"], writes=["gtp"])
            gtT = bd.sb(st, "gtTp", (NE, PT), F32)
            s.dma("sp", gtT[:], gatesT_d[:, p0:p0 + PT], reads=["gatesT_d"], writes=["gtTp"])
            bdn = bd.sb(st, "bdn", (NE, D), F32)
            s.dma("sp", bdn[:], g["bdn_e"][l], writes=["bdn"])
            bgu = bd.sb(st, "bgu", (128, NE, 16), F32)
            s.dma("sp", bgu[:].rearrange("p e c -> p (e c)"), g["bgu_e"][l], writes=["bgu"])
            yac = bd.sb(st, "yac", (128, PT // 128, D), F32)
            st_in = ExitStack()
            act = [bd.sb(st_in, f"actm{i}", (128, 8, PT), BF16) for i in range(1)] * 2
            wgu = [bd.sb(st_in, f"wgu{i}", (128, KC, 512), BF16) for i in range(3)]
            wdn = [bd.sb(st_in, f"wdn{i}", (128, 8, 512), BF16) for i in range(2)]
            tg = [bd.sb(st_in, f"tg{i}", (128, 512), F32) for i in range(1)] * 2
            tl = [bd.sb(st_in, f"tl{i}", (128, 512), F32) for i in range(1)] * 2
            tsg = [bd.sb(st_in, f"tsg{i}", (128, 512), F32) for i in range(1)] * 2
            for ti in range(PT // 128):
                for cb4 in range(4):
                    ps, pk = bd.next_ps()
                    s.mm(ps[:, :], [(gtT[:, ti * 128:(ti + 1) * 128], bdn[:, cb4 * 512:(cb4 + 1) * 512])], reads=["gtTp", "bdn"], writes=[pk])
                    s.op("act", lambda ps=ps, ti=ti, cb4=cb4: nc.scalar.copy(yac[:, ti, cb4 * 512:(cb4 + 1) * 512], ps[:, :]), reads=[pk], writes=[("yac", ti)])
            cnt = [0]
            for j in range(4):
                for r in range(NCORES):
                    e = 4 * r + j
                    Wgu = Wg[("gu", l, j)][r * D:(r + 1) * D, :]
                    Wdn = Wg[("dn", l, j)][r * DE:(r + 1) * DE, :]
                    ab = act[0]
                    ak = ("actm", 0)
                    for mg in range(4):
                        slot = bd.wslot["wgu"] % 3
                        bd.wslot["wgu"] += 1
                        wt = wgu[slot]
                        wk = ("wgu", slot)
                        bd.wload(wt[:, :, 0:256], Wgu[:, mg * 256:(mg + 1) * 256], wk, "w_gu_%d_%d_all" % (l, j))
                        bd.wload(wt[:, :, 256:512], Wgu[:, DE + mg * 256:DE + (mg + 1) * 256], wk, "w_gu_%d_%d_all" % (l, j))
                        for mm_ in range(2):
                            m = mg * 2 + mm_
                            for (t0, tn) in blocks:
                                pg, kg = bd.next_ps()
                                pl, kl = bd.next_ps()
                                s.mm(pg[:, 0:tn], [(wt[:, kc, mm_ * 128:(mm_ + 1) * 128], h2[:, kc, t0 - p0:t0 - p0 + tn]) for kc in range(KC)], reads=[wk, "h2p"], writes=[kg])
                                s.mm(pl[:, 0:tn], [(wt[:, kc, 256 + mm_ * 128:256 + (mm_ + 1) * 128], h2[:, kc, t0 - p0:t0 - p0 + tn]) for kc in range(KC)], reads=[wk, "h2p"], writes=[kl])
                                i2 = 0
                                a_, b_, c_ = tg[i2], tl[i2], tsg[i2]
                                s.op("dve", lambda pg=pg, a_=a_, tn=tn, e=e, m=m: nc.vector.tensor_scalar(a_[:, 0:tn], pg[:, 0:tn], bgu[:, e, m:m + 1], 7.0, ALU.add, ALU.min),
                                     reads=[kg, "bgu"], writes=[("tg", i2)])
                                s.op("act", lambda a_=a_, c_=c_, tn=tn: nc.scalar.activation(c_[:, 0:tn], a_[:, 0:tn], AF.Sigmoid, scale=1.702),
                                     reads=[("tg", i2)], writes=[("tsg", i2)])
                                s.op("dve", lambda pl=pl, b_=b_, tn=tn, e=e, m=m: nc.vector.tensor_scalar(b_[:, 0:tn], pl[:, 0:tn], bgu[:, e, 8 + m:9 + m], 7.0, ALU.add, ALU.min),
                                     reads=[kl, "bgu"], writes=[("tl", i2)])
                                s.op("dve", lambda b_=b_, tn=tn: nc.vector.tensor_scalar(b_[:, 0:tn], b_[:, 0:tn], -7.0, 1.0, ALU.max, ALU.add),
                                     reads=[("tl", i2)], writes=[("tl", i2)])
                                s.op("dve", lambda a_=a_, c_=c_, tn=tn: nc.vector.tensor_tensor(a_[:, 0:tn], a_[:, 0:tn], c_[:, 0:tn], ALU.mult),
                                     reads=[("tg", i2), ("tsg", i2)], writes=[("tg", i2)])
                                s.op("dve", lambda a_=a_, b_=b_, tn=tn, t0=t0, m=m, ab=ab: nc.vector.tensor_tensor(ab[:, m, t0 - p0:t0 - p0 + tn], a_[:, 0:tn], b_[:, 0:tn], ALU.mult),
                                     reads=[("tg", i2), ("tl", i2)], writes=[ak])
                    for cb4 in range(4):
                        slot = bd.wslot["wdn"] % 2
                        bd.wslot["wdn"] += 1
                        wt = wdn[slot]
                        wk = ("wdn", slot)
                        bd.wload(wt[:], Wdn[:, cb4 * 512:(cb4 + 1) * 512], wk, "w_dn_%d_%d_all" % (l, j))
                        for ti in range(PT // 128):
                            ps, pk = bd.next_ps()
                            s.mm(ps[:, :], [(ab[:, kc, ti * 128:(ti + 1) * 128], wt[:, kc, :]) for kc in range(8)], reads=[ak, wk], writes=[pk])
                            s.op("dve", lambda ps=ps, ti=ti, cb4=cb4, e=e: nc.vector.scalar_tensor_tensor(
                                yac[:, ti, cb4 * 512:(cb4 + 1) * 512], ps[:, :], gt[:, ti, e:e + 1], yac[:, ti, cb4 * 512:(cb4 + 1) * 512], ALU.mult, ALU.add),
                                reads=[pk, "gtp", ("yac", ti)], writes=[("yac", ti)])
            s.barrier()
            st_in.close()
            g_bc = bd.sb(st, "g_bc", (128, 2, D), F32)
            for v in range(2):
                s.dma("sp", g_bc[:, v, :], bcast_rows(loc["modsel"][v:v + 1, 5 * 2048:6 * 2048]), reads=["modsel"], writes=["g_bc"])
            nfin_sb = bd.sb(st, "nfin_sb", (128, D), F32)
            s.dma("sp", nfin_sb[:], g["nfin_e"], writes=["nfin"])
            tl = [bd.sb(st, "tlr", (128, 8), F32)] * 2
            xr = [bd.sb(st, f"xr{i}", (128, D), F32) for i in range(1)] * 2
            for ti in range(PT // 128):
                t = p0 // 128 + ti
                xb = xr[ti % 2]
                xk = ("xr", 0)
                v = 1 if t < 2 else 0
                s.dma("sp", xb[:], xmid[t * 128:(t + 1) * 128, :], reads=["xmid"], writes=[xk])
                s.op("dve", lambda ti=ti, v=v: nc.vector.tensor_tensor(yac[:, ti, :], yac[:, ti, :], g_bc[:, v, :], ALU.mult), reads=[("yac", ti), "g_bc"], writes=[("yac", ti)])
                s.op("dve", lambda ti=ti, xb=xb: nc.vector.tensor_tensor(xb[:], xb[:], yac[:, ti, :], ALU.add), reads=[("yac", ti), xk], writes=[xk])
                if not last:
                    s.dma("sp", xcur[t * 128:(t + 1) * 128, :], xb[:], reads=[xk], writes=["xcur"])
                    if t >= 2:
                        s.dma("sp", g["xown"][(t - 2) * 128:(t - 1) * 128, :], xb[:], reads=[xk], writes=["xown"])
                elif t >= 2:
                    s.op("act", lambda xb=xb: nc.scalar.activation(yac[:, ti, :], xb[:], AF.Square), reads=[xk, ("yac", ti)], writes=[("yac", ti)])
                    s.op("dve", lambda ti=ti: nc.vector.reduce_sum(tl[0][:, 0:1], yac[:, ti, :], axis=AX.X), reads=[("yac", ti)], writes=[("tl", 0)])
                    s.op("dve", lambda: nc.vector.tensor_scalar(tl[0][:, 1:2], tl[0][:, 0:1], 1.0 / D, EPS, ALU.mult, ALU.add), reads=[("tl", 0)], writes=[("tl", 0)])
                    s.op("act", lambda: nc.scalar.activation(tl[0][:, 2:3], tl[0][:, 1:2], AF.Sqrt), reads=[("tl", 0)], writes=[("tl", 0)])
                    s.op("dve", lambda: nc.vector.reciprocal(tl[0][:, 3:4], tl[0][:, 2:3]), reads=[("tl", 0)], writes=[("tl", 0)])
                    s.op("dve", lambda xb=xb: nc.vector.tensor_scalar(xb[:], xb[:], tl[0][:, 3:4], None, ALU.mult), reads=[xk, ("tl", 0)], writes=[xk])
                    s.op("dve", lambda xb=xb: nc.vector.tensor_tensor(xb[:], xb[:], nfin_sb[:], ALU.mult), reads=[xk, "nfin"], writes=[xk])
                    s.dma("sp", g["out_e"][(t - 2) * 128:(t - 1) * 128, :], xb[:], reads=[xk], writes=["out"])
            s.barrier()

    if not last:
        s.allgather(g["xown"], g["xgath"], reads=["xown"], writes=["xgath"])
        with ExitStack() as st:
            sel8 = bd.sb(st, "sel8", (128, 8), F32)
            s.dma("sp", sel8[:], g["sel8_e"], writes=["sel8"])
            xa = bd.sb(st, "xa", (128, D), F32)
            xb2 = [bd.sb(st, f"xg{i}", (128, D), F32) for i in range(2)]
            n = 0
            for t in range(HALF // 128):
                for r in range(NCORES):
                    xb = xb2[n % 2]
                    xk = ("xg", n % 2)
                    n += 1
                    s.dma("sp", xb[:], g["xgath"][r * HALF + t * 128:r * HALF + (t + 1) * 128, :], reads=["xgath", xk], writes=[xk])
                    if r == 0:
                        s.op("dve", lambda xb=xb, r=r: nc.vector.tensor_scalar(xa[:], xb[:], sel8[:, r:r + 1], None, ALU.mult), reads=[xk, "sel8", "xa"], writes=["xa"])
                    else:
                        s.op("dve", lambda xb=xb, r=r: nc.vector.scalar_tensor_tensor(xa[:], xb[:], sel8[:, r:r + 1], xa[:], ALU.mult, ALU.add), reads=[xk, "sel8", "xa"], writes=["xa"])
                s.dma("sp", xcur[TOWN + t * 128:TOWN + (t + 1) * 128, :], xa[:], reads=["xa"], writes=["xcur"])
            s.barrier()


def _pp(v, nch):
    return np.ascontiguousarray(v.reshape(nch, 128).T)


_CACHE = {}


def _consts():
    if "c" in _CACHE:
        return _CACHE["c"]
    bf = ml_dtypes.bfloat16
    c = {}
    c["ident_bf"] = np.eye(128, dtype=np.float32).astype(bf)
    c["ident_f32"] = np.eye(128, dtype=np.float32)
    t = np.arange(SEQ)
    row, col = (t // 64).astype(np.float64), (t % 64).astype(np.float64)
    inv = 10000.0 ** (-np.arange(0, 32, 2, dtype=np.float64) / 32)
    ang = np.concatenate([row[:, None] * inv, col[:, None] * inv], axis=-1)
    cos, sin = np.cos(ang).astype(np.float32), np.sin(ang).astype(np.float32)
    c["cos"], c["sin"] = cos, sin
    m = np.arange(256, dtype=np.float64)
    angc = 2 * np.pi * np.outer(m, m) / 256
    c["chC"] = (np.cos(angc) / 16).astype(np.float32).astype(bf)
    c["chSn"] = (-np.sin(angc) / 16).astype(np.float32).astype(bf)
    c["cposC"] = (np.cos(angc) / 16).astype(np.float32).astype(bf)
    c["cposS"] = (np.sin(angc) / 16).astype(np.float32).astype(bf)
    n = np.arange(SEQ, dtype=np.float64)
    angp = 2 * np.pi * np.outer(n, n) / SEQ
    c["posC"] = (np.cos(angp) / np.sqrt(SEQ)).astype(np.float32)
    c["posS"] = (np.sin(angp) / np.sqrt(SEQ)).astype(np.float32)
    _CACHE["c"] = c
    return c


def _core_inputs(core, inp, bd):
    bf = ml_dtypes.bfloat16
    c = _consts()
    b, h = core // 2, core % 2
    own = slice(h * HALF, (h + 1) * HALF)
    oth = slice((1 - h) * HALF, (2 - h) * HALF)
    m = {}
    m["x_loc"] = np.concatenate([inp["ctx"][b], inp["x"][b][own], inp["x"][b][oth]], axis=0)
    cv = np.concatenate([inp["c"], inp["c_ctx"][None, :]], axis=0)
    m["cT"] = np.ascontiguousarray(cv.reshape(5, KC, 128).transpose(2, 1, 0)).reshape(128, KC * 5)
    oh = np.zeros((128, 4), np.float32); oh[:, b] = 1.0
    m["onehot_b"] = oh
    s8 = np.zeros((128, 8), np.float32); s8[:, core ^ 1] = 1.0
    m["sel8"] = s8
    order = np.concatenate([np.arange(SEQ)[own], np.arange(SEQ)[oth]])
    cosl, sinl = c["cos"][order], c["sin"][order]
    rc = np.ones((64, TS), np.float32); rs = np.zeros((64, TS), np.float32)
    rc[0:32, CTX:] = cosl.T; rc[32:64, CTX:] = cosl.T
    rs[0:32, CTX:] = -sinl.T; rs[32:64, CTX:] = sinl.T
    m["ropeC"], m["ropeS"] = rc, rs
    hm = np.zeros((128, 2), np.float32); hm[:, 0] = float(h == 1); hm[:, 1] = float(h == 0)
    m["halo_mask"] = hm
    m["posC"] = np.ascontiguousarray(c["posC"][order][:, own]).astype(bf)
    m["posS"] = np.ascontiguousarray(c["posS"][order][:, own]).astype(bf)
    for k in ("cposC", "cposS", "chC", "chSn", "ident_bf", "ident_f32"):
        m[k] = c[k]
    m["norm1_pp"] = np.stack([_pp(inp["norm1"][l], KC) for l in range(L)])
    m["norm2_pp"] = np.stack([_pp(inp["norm2"][l], KC) for l in range(L)])
    m["qnorm_pp"] = np.stack([_pp(inp["q_norm"][l], 4) for l in range(L)])
    m["kvnorm_pp"] = np.stack([_pp(inp["kv_norm"][l], 4) for l in range(L)])
    m["conv_dw_pp"] = np.stack([np.ascontiguousarray(inp["conv_dw"][l].reshape(31, 8, 128).transpose(2, 1, 0)).reshape(128, 8 * 31) for l in range(L)])
    m["conv_b_pp"] = np.stack([_pp(inp["conv_dw_b"][l], 8) for l in range(L)])
    m["conv_lng_pp"] = np.stack([_pp(inp["conv_ln_g"][l], 8) for l in range(L)])
    m["conv_lnb_pp"] = np.stack([_pp(inp["conv_ln_b"][l], 8) for l in range(L)])
    m["b_gu_pp"] = np.stack([np.ascontiguousarray(inp["b_gate_up"][l].reshape(NE, 16, 128).transpose(2, 0, 1)).reshape(128, NE * 16) for l in range(L)])
    m["b_router_bc"] = np.stack([np.broadcast_to(inp["b_router"][l][None, :], (128, NE)).copy() for l in range(L)])
    m["b_down"] = np.ascontiguousarray(inp["b_down"])
    m["w_router_pp"] = np.stack([np.ascontiguousarray(inp["w_router"][l].reshape(KC, 128, NE).transpose(1, 0, 2)).reshape(128, KC * NE) for l in range(L)])
    m["norm_final_bc"] = np.broadcast_to(inp["norm_final"][None, :], (128, D)).copy()
    m["b_ada_sh"] = np.ascontiguousarray(inp["b_ada"][:, None, core * ADA_SH:(core + 1) * ADA_SH])
    m["w_ada_sh"] = np.ascontiguousarray(inp["w_ada"][:, :, core * ADA_SH:(core + 1) * ADA_SH])
    for l in range(bd.nlayers):
        for nm in ("w_in", "w_uq", "w_ukv", "w_mla_out", "w_conv_out", "w_four_out", "w_out"):
            W = inp[nm][l]
            r = W.shape[0] // 8
            m[f"{nm}_{l}"] = np.ascontiguousarray(W[core * r:(core + 1) * r])
        if bd.with_moe:
            for j in range(4):
                m[f"w_gu_{l}_{j}"] = np.ascontiguousarray(inp["w_gate_up"][l, 4 * core + j])
                m[f"w_dn_{l}_{j}"] = np.ascontiguousarray(inp["w_down"][l, 4 * core + j])
    out = {}
    for k, (shape, dt) in bd.inputs.items():
        a = m[k]
        assert tuple(a.shape) == tuple(shape), (k, a.shape, shape)
        out[k] = a
    return out


def kernel(**inputs):
    inp = {k: np.asarray(v) for k, v in inputs.items()}
    bd = Builder()
    bd.stop_after = None
    build_program(bd)
    in_maps = [_core_inputs(c, inp, bd) for c in range(NCORES)]
    res = run_bass_kernel_spmd(bd.nc, in_maps, core_ids=list(range(NCORES)))
    out = np.zeros((B, SEQ, D), np.float32)
    for c in range(NCORES):
        b, h = c // 2, c % 2
        out[b, h * HALF:(h + 1) * HALF] = res.results[c]["out"]
    return out
```
